# Optimizing a Trainium2 kernel written in Bass

```python
import jax, jax.numpy as jnp
from jax import lax
import numpy as np

D_MODEL = 1024
BATCH = 4
SEQ = 8192
DEPTH = 2

N_A_LAYERS = DEPTH // 2
N_B_LAYERS = DEPTH - N_A_LAYERS
N_DENSE_LAYERS = (DEPTH + 1) // 2
N_MOE_LAYERS = DEPTH // 2

LRU_WIDTH = D_MODEL
LRU_BLOCKS = 4
LRU_BLOCK_W = LRU_WIDTH // LRU_BLOCKS
CONV_WIDTH = 4
LRU_C = 8.0

HEAD_DIM = 64
N_Q_HEADS = D_MODEL // HEAD_DIM
N_KV_HEADS = 2
GROUP = N_Q_HEADS // N_KV_HEADS
WINDOW = 128
BLOCK = 128
ROPE_THETA = 500000.0
ROT_DIM = HEAD_DIM // 4

D_FF = 3 * D_MODEL
N_EXPERTS = 8
TOP_K = 2
D_FF_EXPERT = D_MODEL

PLE_DIM = 256
EPS = 1e-6

kernel_name = 'yoco_rglru_swa_sink_moe_trunk'


def rmsnorm(x, g):
    xf = x.astype(jnp.float32)
    y = xf * lax.rsqrt(jnp.mean(xf * xf, axis=-1, keepdims=True) + EPS) * g.astype(jnp.float32)
    return y.astype(x.dtype)


def rope_tables(positions):
    freqs = ROPE_THETA ** (-jnp.arange(0, ROT_DIM, 2, dtype=jnp.float32) / ROT_DIM)
    ang = positions.astype(jnp.float32)[..., None] * freqs
    return jnp.cos(ang)[:, :, None, :], jnp.sin(ang)[:, :, None, :]


def partial_rope(t, cos, sin):
    half = ROT_DIM // 2
    t1 = t[..., :half].astype(jnp.float32)
    t2 = t[..., half:ROT_DIM].astype(jnp.float32)
    rot = jnp.concatenate([t1 * cos - t2 * sin, t2 * cos + t1 * sin], axis=-1).astype(t.dtype)
    return jnp.concatenate([rot, t[..., ROT_DIM:]], axis=-1)


def _lru_combine(c1, c2):
    a1, b1 = c1
    a2, b2 = c2
    return a1 * a2, a2 * b1 + b2


def rglru_block(xn, w_in, conv_w, conv_b, w_ga, b_ga, w_gx, b_gx, lam, w_out):
    B, S, _ = xn.shape
    h = xn @ w_in
    gate_branch, xb = jnp.split(h, 2, axis=-1)
    gate = jax.nn.gelu(gate_branch)
    xpad = jnp.pad(xb, ((0, 0), (CONV_WIDTH - 1, 0), (0, 0)))
    xc = conv_b
    for k in range(CONV_WIDTH):
        xc = xc + conv_w[k] * xpad[:, CONV_WIDTH - 1 - k: CONV_WIDTH - 1 - k + S]
    xh = xc.reshape(B, S, LRU_BLOCKS, LRU_BLOCK_W)
    r = jax.nn.sigmoid(jnp.einsum('bshi,hij->bshj', xh, w_ga).reshape(B, S, LRU_WIDTH) + b_ga)
    i = jax.nn.sigmoid(jnp.einsum('bshi,hij->bshj', xh, w_gx).reshape(B, S, LRU_WIDTH) + b_gx)
    log_a = -LRU_C * r.astype(jnp.float32) * jax.nn.softplus(-lam.astype(jnp.float32))
    a = jnp.exp(log_a)
    mult = jnp.sqrt(-jnp.expm1(2.0 * log_a))
    mult = mult.at[:, 0].set(1.0)
    b_in = mult * (i * xc).astype(jnp.float32)
    _, hs = lax.associative_scan(_lru_combine, (a, b_in), axis=1)
    return (hs.astype(xn.dtype) * gate) @ w_out


def _blockify_with_prev(t):
    B, S, Hk, Dh = t.shape
    tb = t.reshape(B, S // BLOCK, BLOCK, Hk, Dh)
    prev = jnp.pad(tb, ((0, 0), (1, 0), (0, 0), (0, 0), (0, 0)))[:, :-1]
    return jnp.concatenate([prev, tb], axis=2)


def sliding_window_attention_sinks(q, k, v, sinks):
    B, S, _, _ = q.shape
    NB = S // BLOCK
    qb = q.reshape(B, NB, BLOCK, N_KV_HEADS, GROUP, HEAD_DIM)
    kk = _blockify_with_prev(k)
    vv = _blockify_with_prev(v)
    scores = jnp.einsum('bnqhgd,bnjhd->bnhgqj', qb, kk).astype(jnp.float32) * (HEAD_DIM ** -0.5)
    qi = jnp.arange(BLOCK)[:, None]
    kj = jnp.arange(2 * BLOCK)[None, :]
    diff = qi + BLOCK - kj
    band = (diff >= 0) & (diff < WINDOW)
    valid = (jnp.arange(NB)[:, None, None] > 0) | (kj >= BLOCK)[None]
    mask = (band[None] & valid)[None, :, None, None]
    scores = jnp.where(mask, scores, jnp.float32(-1e30))
    sink = jnp.broadcast_to(sinks.astype(jnp.float32).reshape(1, 1, N_KV_HEADS, GROUP, 1, 1),
                            scores.shape[:-1] + (1,))
    probs = jax.nn.softmax(jnp.concatenate([scores, sink], axis=-1), axis=-1)[..., :-1]
    out = jnp.einsum('bnhgqj,bnjhd->bnqhgd', probs.astype(v.dtype), vv)
    return out.reshape(B, S, N_Q_HEADS * HEAD_DIM)


def swiglu(xn, w_in, w_out):
    g, u = jnp.split(xn @ w_in, 2, axis=-1)
    return (jax.nn.silu(g) * u) @ w_out


def moe_swiglu(xn, w_router, b_router, w_in, w_out):
    logits = (xn @ w_router).astype(jnp.float32) + b_router.astype(jnp.float32)
    top_vals, top_idx = lax.top_k(logits, TOP_K)
    top_w = jax.nn.softmax(top_vals, axis=-1)
    gates = jnp.einsum('bske,bsk->bse', jax.nn.one_hot(top_idx, N_EXPERTS, dtype=jnp.float32), top_w)
    y = jnp.zeros_like(xn)
    for e in range(N_EXPERTS):
        y = y + gates[..., e:e + 1].astype(xn.dtype) * swiglu(xn, w_in[e], w_out[e])
    return y


def setup_inputs(seed: int = 0) -> dict:
    key = jax.random.key(seed)
    ks = iter(jax.random.split(key, 48))
    f32 = jnp.float32

    def nrm(shape, fan_in):
        return jax.random.normal(next(ks), shape, f32) * (fan_in ** -0.5)

    def gain(shape):
        return 1.0 + 0.02 * jax.random.normal(next(ks), shape, f32)

    def bias(shape):
        return 0.01 * jax.random.normal(next(ks), shape, f32)

    x = jax.random.normal(next(ks), (BATCH, SEQ, D_MODEL), f32)
    p = jax.random.normal(next(ks), (DEPTH, BATCH, SEQ, PLE_DIM), f32)
    offsets = jax.random.randint(next(ks), (BATCH, 1), 0, 4096, dtype=jnp.int32)
    positions = offsets + jnp.arange(SEQ, dtype=jnp.int32)[None, :]

    a0 = jax.random.uniform(next(ks), (N_A_LAYERS, LRU_WIDTH), f32, 0.9, 0.999) ** (1.0 / LRU_C)
    lru_lambda = jnp.log(a0) - jnp.log1p(-a0)

    return {
        'x': x,
        'p': p,
        'positions': positions,
        'g_mix': gain((DEPTH, D_MODEL)),
        'g_ffn': gain((DEPTH, D_MODEL)),
        'g_ple': gain((DEPTH, D_MODEL)),
        'w_ple_gate': nrm((DEPTH, D_MODEL, D_MODEL), D_MODEL),
        'w_ple_proj': nrm((DEPTH, PLE_DIM, D_MODEL), PLE_DIM),
        'w_lru_in': nrm((N_A_LAYERS, D_MODEL, 2 * LRU_WIDTH), D_MODEL),
        'lru_conv_w': nrm((N_A_LAYERS, CONV_WIDTH, LRU_WIDTH), CONV_WIDTH),
        'lru_conv_b': bias((N_A_LAYERS, LRU_WIDTH)),
        'w_lru_ga': nrm((N_A_LAYERS, LRU_BLOCKS, LRU_BLOCK_W, LRU_BLOCK_W), LRU_BLOCK_W),
        'b_lru_ga': bias((N_A_LAYERS, LRU_WIDTH)),
        'w_lru_gx': nrm((N_A_LAYERS, LRU_BLOCKS, LRU_BLOCK_W, LRU_BLOCK_W), LRU_BLOCK_W),
        'b_lru_gx': bias((N_A_LAYERS, LRU_WIDTH)),
        'lru_lambda': lru_lambda,
        'w_lru_out': nrm((N_A_LAYERS, LRU_WIDTH, D_MODEL), LRU_WIDTH),
        'g_kv': gain((D_MODEL,)),
        'w_kv': nrm((D_MODEL, 2 * N_KV_HEADS * HEAD_DIM), D_MODEL),
        'w_q': nrm((N_B_LAYERS, D_MODEL, N_Q_HEADS * HEAD_DIM), D_MODEL),
        'attn_sinks': jax.random.normal(next(ks), (N_B_LAYERS, N_Q_HEADS), f32),
        'w_o': nrm((N_B_LAYERS, N_Q_HEADS * HEAD_DIM, D_MODEL), N_Q_HEADS * HEAD_DIM),
        'w_ff_in': nrm((N_DENSE_LAYERS, D_MODEL, 2 * D_FF), D_MODEL),
        'w_ff_out': nrm((N_DENSE_LAYERS, D_FF, D_MODEL), D_FF),
        'w_router': nrm((N_MOE_LAYERS, D_MODEL, N_EXPERTS), D_MODEL),
        'b_router': bias((N_MOE_LAYERS, N_EXPERTS)),
        'w_exp_in': nrm((N_MOE_LAYERS, N_EXPERTS, D_MODEL, 2 * D_FF_EXPERT), D_MODEL),
        'w_exp_out': nrm((N_MOE_LAYERS, N_EXPERTS, D_FF_EXPERT, D_MODEL), D_FF_EXPERT),
        'g_final': gain((D_MODEL,)),
    }


def reference(x, p, positions, g_mix, g_ffn, g_ple, w_ple_gate, w_ple_proj,
              w_lru_in, lru_conv_w, lru_conv_b, w_lru_ga, b_lru_ga, w_lru_gx, b_lru_gx,
              lru_lambda, w_lru_out, g_kv, w_kv, w_q, attn_sinks, w_o,
              w_ff_in, w_ff_out, w_router, b_router, w_exp_in, w_exp_out, g_final):
    B, S, _ = x.shape
    cos, sin = rope_tables(positions)
    k_shared = None
    v_shared = None
    for i in range(DEPTH):
        xn = rmsnorm(x, g_mix[i])
        if i < N_A_LAYERS:
            a = i
            x = x + rglru_block(xn, w_lru_in[a], lru_conv_w[a], lru_conv_b[a], w_lru_ga[a], b_lru_ga[a],
                                w_lru_gx[a], b_lru_gx[a], lru_lambda[a], w_lru_out[a])
        else:
            b = i - N_A_LAYERS
            q = partial_rope((xn @ w_q[b]).reshape(B, S, N_Q_HEADS, HEAD_DIM), cos, sin)
            attn = sliding_window_attention_sinks(q, k_shared, v_shared, attn_sinks[b])
            x = x + attn @ w_o[b]
        xn = rmsnorm(x, g_ffn[i])
        if i % 2 == 0:
            x = x + swiglu(xn, w_ff_in[i // 2], w_ff_out[i // 2])
        else:
            m = i // 2
            x = x + moe_swiglu(xn, w_router[m], b_router[m], w_exp_in[m], w_exp_out[m])
        ple_gate = jax.nn.sigmoid(rmsnorm(x, g_ple[i]) @ w_ple_gate[i])
        x = x + ple_gate * (p[i] @ w_ple_proj[i])
        if i == N_A_LAYERS - 1:
            kv = rmsnorm(x, g_kv) @ w_kv
            k_flat, v_flat = jnp.split(kv, 2, axis=-1)
            k_shared = partial_rope(k_flat.reshape(B, S, N_KV_HEADS, HEAD_DIM), cos, sin)
            v_shared = v_flat.reshape(B, S, N_KV_HEADS, HEAD_DIM)
    return rmsnorm(x, g_final)
```

```python
import contextlib
import numpy as np
import concourse.bass as bass
import concourse.mybir as mybir
from concourse.bass_utils import run_bass_kernel_spmd

F32 = mybir.dt.float32
BF16 = mybir.dt.bfloat16
I32 = mybir.dt.int32
U8 = mybir.dt.uint8
AF = mybir.ActivationFunctionType
ALU = mybir.AluOpType
AX = mybir.AxisListType

ENGS = ("pe", "act", "dve", "pool", "sp")
T = 512
D = 1024
NC8 = 8
EPS = 1e-6
NSLOT = 5
SLOT_ELEMS = 4096
PI = float(np.pi)
import os
PSUM_NORM = os.environ.get("K_PSUM_NORM", "0") == "1"


class Buf:
    __slots__ = ("name", "w", "r")

    def __init__(self, name):
        self.name = name
        self.w = None
        self.r = {}


class Op:
    __slots__ = ("eng", "fn", "deps", "signal", "count", "dma_sem", "dma_count")

    def __init__(self, eng, fn):
        self.eng = eng
        self.fn = fn
        self.deps = []
        self.signal = False
        self.count = 0
        self.dma_sem = None
        self.dma_count = 0


class Prog:
    def __init__(self, nc):
        self.nc = nc
        self.ops = {e: [] for e in ENGS}
        self.dma_sems = {}
        self.all_ops = []

    def op(self, eng, fn, reads=(), writes=(), dma_key=None, extra_deps=()):
        o = Op(eng, fn)
        deps = []
        for b in reads:
            if b.w is not None:
                deps.append((b.w, True))
        for b in writes:
            if b.w is not None:
                deps.append((b.w, False))
            for r in b.r.values():
                deps.append((r, False))
        for d in extra_deps:
            deps.append((d, True))
        seen = set()
        for d, raw in deps:
            if d is o or id(d) in seen:
                continue
            if d.dma_sem is None and d.eng == eng and (not raw or eng in ("pe", "sp")):
                continue
            seen.add(id(d))
            o.deps.append(d)
        is_dma = dma_key is not None
        for b in reads:
            b.r[(eng, id(o)) if is_dma else eng] = o
        for b in writes:
            b.w = o
            b.r = {}
        if is_dma:
            ent = self.dma_sems.setdefault(dma_key, [len(self.dma_sems), 0])
            ent[1] += 16
            o.dma_sem = ent[0]
            o.dma_count = ent[1]
        self.ops[eng].append(o)
        self.all_ops.append(o)
        return o

    def emit(self, final_waits=()):
        nc = self.nc
        for o in self.all_ops:
            for d in o.deps:
                if d.dma_sem is None:
                    d.signal = True
        for o in final_waits:
            if o.dma_sem is None:
                o.signal = True
        for e in ENGS:
            c = 0
            for o in self.ops[e]:
                if o.signal:
                    c += 1
                    o.count = c
        with contextlib.ExitStack() as st:
            esem = {e: st.enter_context(nc.semaphore("s_" + e)) for e in ENGS}
            dsem = [st.enter_context(nc.semaphore("d_%d" % i)) for i in range(len(self.dma_sems))]
            block = st.enter_context(nc.Block())

            def gen(ename):
                def body(eng):
                    waited = {}
                    dwaited = {}
                    for o in self.ops[ename]:
                        for d in o.deps:
                            if d.dma_sem is not None:
                                if dwaited.get(d.dma_sem, 0) < d.dma_count:
                                    eng.wait_ge(dsem[d.dma_sem], d.dma_count)
                                    dwaited[d.dma_sem] = d.dma_count
                            else:
                                if waited.get(d.eng, 0) < d.count:
                                    eng.wait_ge(esem[d.eng], d.count)
                                    waited[d.eng] = d.count
                        ins = o.fn(eng)
                        if o.dma_sem is not None:
                            ins.then_inc(dsem[o.dma_sem], 16)
                        elif o.signal:
                            ins.then_inc(esem[ename], 1)
                    if ename == "sp":
                        for o in final_waits:
                            if o.dma_sem is not None:
                                eng.wait_ge(dsem[o.dma_sem], o.dma_count)
                            else:
                                eng.wait_ge(esem[o.eng], o.count)
                return body

            block.tensor(gen("pe"))
            block.scalar(gen("act"))
            block.vector(gen("dve"))
            block.gpsimd(gen("pool"))
            block.sync(gen("sp"))


def _qcols():
    cols = np.zeros((8, 128), np.int64)
    for c in range(8):
        for p in range(128):
            head = c if p < 64 else 8 + c
            cols[c, p] = head * 64 + (p % 64)
    return cols


def _ropeswap(cols):
    out = cols.copy()
    d = cols % 64
    out = np.where(d < 8, cols + 8, np.where(d < 16, cols - 8, cols))
    return out


def _slab(W, cols):
    K = W.shape[0]
    kc = K // 128
    sub = W[:, cols]
    n = sub.shape[1]
    return np.ascontiguousarray(sub.reshape(kc, 128, n).transpose(1, 0, 2).reshape(128, kc * n))


def build_slabs(inp):
    S = []
    r = np.arange
    w_in = inp["w_lru_in"][0]
    for h in range(2):
        S.append(("in_x%d" % h, _slab(w_in, 1024 + h * 512 + r(512))))
    g = np.stack([inp["w_lru_ga"][0], inp["w_lru_gx"][0]])
    g = g.reshape(2, 4, 2, 128, 256).transpose(3, 0, 1, 2, 4).reshape(128, 4096)
    S.append(("gates", np.ascontiguousarray(g)))
    for h in range(2):
        S.append(("in_g%d" % h, _slab(w_in, h * 512 + r(512))))
    for h in range(2):
        S.append(("out%d" % h, _slab(inp["w_lru_out"][0], h * 512 + r(512))))
    wf = inp["w_ff_in"][0]
    for s in range(12):
        j = 2 * s
        cols = np.concatenate([j * 128 + r(128), (j + 1) * 128 + r(128), 3072 + j * 128 + r(128), 3072 + (j + 1) * 128 + r(128)])
        S.append(("ffin%d" % s, _slab(wf, cols)))
    for m in range(8):
        S.append(("ffout%d" % m, _slab(inp["w_ff_out"][0], m * 128 + r(128))))
    for h in range(2):
        S.append(("pleg0_%d" % h, _slab(inp["w_ple_gate"][0], h * 512 + r(512))))
    S.append(("plep0", _slab(inp["w_ple_proj"][0], r(1024))))
    kcols = r(128)
    S.append(("kv", _slab(inp["w_kv"], np.concatenate([kcols, _ropeswap(kcols), 128 + r(128)]))))
    qc = _qcols()
    wq = inp["w_q"][0]
    for s in range(4):
        c0, c1 = 2 * s, 2 * s + 1
        cols = np.concatenate([qc[c0], _ropeswap(qc[c0]), qc[c1], _ropeswap(qc[c1])])
        S.append(("q%d" % s, _slab(wq, cols)))
    wo = inp["w_o"][0][qc.reshape(-1), :]
    for h in range(2):
        S.append(("o%d" % h, _slab(wo, h * 512 + r(512))))
    for e in range(8):
        we = inp["w_exp_in"][0][e]
        for s in range(4):
            j = 2 * s
            cols = np.concatenate([j * 128 + r(128), (j + 1) * 128 + r(128), 1024 + j * 128 + r(128), 1024 + (j + 1) * 128 + r(128)])
            S.append(("ein%d_%d" % (e, s), _slab(we, cols)))
        for h in range(2):
            S.append(("eout%d_%d" % (e, h), _slab(inp["w_exp_out"][0][e], h * 512 + r(512))))
    for h in range(2):
        S.append(("pleg1_%d" % h, _slab(inp["w_ple_gate"][1], h * 512 + r(512))))
    S.append(("plep1", _slab(inp["w_ple_proj"][1], r(1024))))
    return S


def slab_table():
    names = []
    for h in range(2):
        names.append(("in_x%d" % h, 4096))
    names.append(("gates", 4096))
    for h in range(2):
        names.append(("in_g%d" % h, 4096))
    for h in range(2):
        names.append(("out%d" % h, 4096))
    for s in range(12):
        names.append(("ffin%d" % s, 4096))
    for m in range(8):
        names.append(("ffout%d" % m, 3072))
    for h in range(2):
        names.append(("pleg0_%d" % h, 4096))
    names.append(("plep0", 2048))
    names.append(("kv", 3072))
    for s in range(4):
        names.append(("q%d" % s, 4096))
    for h in range(2):
        names.append(("o%d" % h, 4096))
    for e in range(8):
        for s in range(4):
            names.append(("ein%d_%d" % (e, s), 4096))
        for h in range(2):
            names.append(("eout%d_%d" % (e, h), 4096))
    for h in range(2):
        names.append(("pleg1_%d" % h, 4096))
    names.append(("plep1", 2048))
    tab = {}
    off = 0
    for nm, n in names:
        tab[nm] = (off, n)
        off += 128 * n
    return tab, off


VC = {}
_o = 0
for _nm, _n in [("g_mix0", 8), ("g_ffn0", 8), ("g_ple0", 8), ("g_kv", 8), ("g_mix1", 8), ("g_ffn1", 8),
                ("g_ple1", 8), ("g_final", 8), ("conv_w", 32), ("conv_b", 8), ("b_ga", 8), ("b_gx", 8),
                ("lam", 8), ("freq", 1), ("sign", 1), ("carry", 1), ("omc", 1)]:
    VC[_nm] = _o
    _o += _n
NV = _o


def build_vecs(inp, carry):
    v = np.zeros((128, NV), np.float32)

    def col(x):
        return np.asarray(x, np.float32).reshape(8, 128).T

    v[:, VC["g_mix0"]:VC["g_mix0"] + 8] = col(inp["g_mix"][0])
    v[:, VC["g_ffn0"]:VC["g_ffn0"] + 8] = col(inp["g_ffn"][0])
    v[:, VC["g_ple0"]:VC["g_ple0"] + 8] = col(inp["g_ple"][0])
    v[:, VC["g_kv"]:VC["g_kv"] + 8] = col(inp["g_kv"])
    v[:, VC["g_mix1"]:VC["g_mix1"] + 8] = col(inp["g_mix"][1])
    v[:, VC["g_ffn1"]:VC["g_ffn1"] + 8] = col(inp["g_ffn"][1])
    v[:, VC["g_ple1"]:VC["g_ple1"] + 8] = col(inp["g_ple"][1])
    v[:, VC["g_final"]:VC["g_final"] + 8] = col(inp["g_final"])
    for k in range(4):
        v[:, VC["conv_w"] + 8 * k:VC["conv_w"] + 8 * k + 8] = col(inp["lru_conv_w"][0][k])
    v[:, VC["conv_b"]:VC["conv_b"] + 8] = col(inp["lru_conv_b"][0])
    v[:, VC["b_ga"]:VC["b_ga"] + 8] = col(inp["b_lru_ga"][0])
    v[:, VC["b_gx"]:VC["b_gx"] + 8] = col(inp["b_lru_gx"][0])
    v[:, VC["lam"]:VC["lam"] + 8] = col(inp["lru_lambda"][0])
    freqs = (np.float32(500000.0) ** (-np.arange(0, 16, 2, dtype=np.float32) / np.float32(16))).astype(np.float32)
    for p in range(128):
        d = p % 64
        if d < 16:
            v[p, VC["freq"]] = freqs[d % 8]
            v[p, VC["sign"]] = -1.0 if d < 8 else 1.0
    v[:, VC["carry"]] = carry
    v[:, VC["omc"]] = 1.0 - carry
    return v


def build_program(NPRE, NREAL, taps=()):
    nc = bass.Bass("TRN2", target_bir_lowering=False)
    P = Prog(nc)
    NT_ALL = NPRE + NREAL
    TOK = NT_ALL * T
    TOKP = (1 + NREAL) * T
    stab, wtotal = slab_table()
    WROWS = wtotal // 2048

    xT = nc.dram_tensor("xT", [D, TOK], F32, kind="ExternalInput")
    pT = nc.dram_tensor("pT", [2, 256, TOKP], F32, kind="ExternalInput")
    posd = nc.dram_tensor("pos", [1, TOKP], I32, kind="ExternalInput")
    wflat = nc.dram_tensor("wflat", [WROWS, 2048], F32, kind="ExternalInput")
    vecsd = nc.dram_tensor("vecs", [128, NV], F32, kind="ExternalInput")
    identd = nc.dram_tensor("ident", [128, 128], F32, kind="ExternalInput")
    maskd = nc.dram_tensor("maskc", [128, 2, 128], F32, kind="ExternalInput")
    sinkd = nc.dram_tensor("sinks", [1, 16], F32, kind="ExternalInput")
    wrd = nc.dram_tensor("wr", [128, 64], F32, kind="ExternalInput")
    brd = nc.dram_tensor("br", [1, 8], F32, kind="ExternalInput")
    outT = nc.dram_tensor("outT", [D, NREAL * T], F32, kind="ExternalOutput")
    wscr = nc.dram_tensor("wscr", [WROWS, 2048], BF16, kind="Internal")
    tapd = {nm: nc.dram_tensor("tap_" + nm, list(shape), F32, kind="ExternalOutput") for nm, shape in taps}

    st = contextlib.ExitStack()

    def sb(name, shape, dt):
        return st.enter_context(nc.sbuf_tensor(name, shape, dt))

    with st:
        X = [sb("X%d" % i, [128, 8, T], F32) for i in range(2)]
        XB_X = [[Buf("X%d_%d" % (i, c)) for c in range(8)] for i in range(2)]
        XN = sb("XN", [128, 8, T], BF16)
        B_XN = [Buf("XN%d" % c) for c in range(8)]
        ring = sb("ring", [128, NSLOT, SLOT_ELEMS], BF16)
        B_ring = [Buf("ring%d" % s) for s in range(NSLOT)]
        PST = [sb("PST%d" % i, [128, 2, 2, T], F32) for i in range(2)]
        B_PST = [Buf("PST%d" % i) for i in range(2)]
        PT = sb("PT", [128, 2, T], BF16)
        B_PT = Buf("PT")
        POSI = [sb("POSI%d" % i, [128, T], I32) for i in range(2)]
        B_POSI = [Buf("POSI%d" % i) for i in range(2)]
        COS = sb("COS", [128, T], F32)
        SIN = sb("SIN", [128, T], F32)
        B_CS = Buf("cossin")
        RT = [sb("RT%d" % i, [128, T], F32) for i in range(3)]
        B_RT = [Buf("RT%d" % i) for i in range(3)]
        RTI = sb("RTI", [128, T], I32)
        B_RTI = Buf("RTI")
        KT = sb("KT", [128, 5 * 128], BF16)
        B_KT = [Buf("KT%d" % i) for i in range(5)]
        VT = sb("VT", [128, 5, 128], BF16)
        B_VT = [Buf("VT%d" % i) for i in range(5)]
        XBh = sb("XBh", [128, 8, 4 + T], BF16)
        B_XBh = [Buf("XBh%d" % c) for c in range(8)]
        RS = sb("RS", [128, T], F32)
        B_RS = Buf("RS")
        dummy = sb("dmy_sq", [128, 2], F32)
        B_dummy = Buf("dummy")
        HS = sb("HS", [128, 8], F32)
        B_HS = [Buf("HS%d" % c) for c in range(8)]
        vecs = sb("vecs_sb", [128, NV], F32)
        B_vecs = Buf("vecs")
        ident = sb("ident_sb", [128, 128], F32)
        B_ident = Buf("ident")
        ones_b = sb("ones_b", [128, 128], BF16)
        ones_f = sb("ones_f", [128, 128], F32)
        B_ones = Buf("ones")
        DG = sb("DG", [128, 8, 4, 128], BF16)
        B_DG = Buf("DG")
        lruc = sb("lruc", [128, 4, 8], F32)
        B_lruc = Buf("lruc")
        maskf = sb("maskf", [128, 2, 128], F32)
        B_mask = Buf("mask")
        sinkb = sb("sinkb", [128, 16], F32)
        ESINK2 = sb("ESINK2", [128, 4, 256], F32)
        ident_b = sb("ident_b", [128, 128], BF16)
        MASKB = sb("MASKB", [128, 2, T], BF16)
        B_esink = Buf("esink")
        wr_f = sb("wr_f", [128, 64], F32)
        wr_b = sb("wr_b", [128, 64], BF16)
        B_wr = Buf("wr")
        brt = sb("brt", [128, 4, 8], F32)
        B_brt = Buf("brt")
        RL = sb("RL", [128, 4, 8], F32)
        RL2 = sb("RL2", [128, 4, 8], F32)
        REQ1 = sb("REQ1", [128, 4, 8], F32)
        REQ2 = sb("REQ2", [128, 4, 8], F32)
        RGT = sb("RGT", [128, 4, 8], F32)
        RM = sb("RM", [128, 4, 4], F32)
        B_rt_small = Buf("router_small")
        DE = [sb("DE%d" % i, [128, 4, 128], F32) for i in range(2)]
        B_DE = [Buf("DE%d" % i) for i in range(2)]
        NPAGE = 24
        AR = sb("AR", [128, NPAGE * 2048], U8)
        B_pg = [Buf("pg%d" % i) for i in range(NPAGE)]

        def pview(page, dt, nelem, off_bytes=0):
            esz = 4 if dt in (F32, I32) else 2
            b0 = page * 2048 + off_bytes
            return AR[:, b0:b0 + nelem * esz].bitcast(dt)

        def f32pg(page):
            return pview(page, F32, T), [B_pg[page]]

        def bf16half(page, half):
            return pview(page, BF16, T, half * 1024), [B_pg[page]]

        banks = [st.enter_context(nc.psum_tensor("bank%d" % i, [128, T], F32)) for i in range(8)]
        B_bank = [Buf("bank%d" % i) for i in range(8)]
        bank_ctr = [0]

        def nb():
            i = bank_ctr[0] % 8
            bank_ctr[0] += 1
            return banks[i], B_bank[i]

        def vcol(name, c=0):
            j = VC[name] + c
            return vecs[:, j:j + 1]

        def mm(out, lhsT, rhs, start, stop, reads, writes):
            return P.op("pe", lambda e: e.matmul(out, lhsT, rhs, start=start, stop=stop), reads=reads, writes=writes)

        def act(out, in_, func, reads, writes, scale=None, bias=None):
            kw = {}
            if scale is not None:
                kw["scale"] = scale
            if bias is not None:
                kw["bias"] = bias
            return P.op("act", lambda e: e.activation(out=out, in_=in_, func=func, **kw), reads=reads, writes=writes)

        def tt(eng, out, in0, in1, op, reads, writes):
            return P.op(eng, lambda e: e.tensor_tensor(out=out, in0=in0, in1=in1, op=op), reads=reads, writes=writes)

        def ts(eng, out, in0, s1, op0, reads, writes, s2=None, op1=None):
            if op1 is None:
                return P.op(eng, lambda e: e.tensor_scalar(out=out, in0=in0, scalar1=s1, scalar2=None, op0=op0), reads=reads, writes=writes)
            return P.op(eng, lambda e: e.tensor_scalar(out=out, in0=in0, scalar1=s1, scalar2=s2, op0=op0, op1=op1), reads=reads, writes=writes)

        def stt(out, in0, scalar, in1, op0, op1, reads, writes):
            return P.op("dve", lambda e: e.scalar_tensor_tensor(out=out, in0=in0, scalar=scalar, in1=in1, op0=op0, op1=op1), reads=reads, writes=writes)

        def cp(eng, out, in_, reads, writes):
            return P.op(eng, lambda e: e.tensor_copy(out=out, in_=in_), reads=reads, writes=writes)

        def memset(eng, ap, val, writes):
            return P.op(eng, lambda e: e.memset(ap, val), writes=writes)

        def scan(out, d0, d1, init, reads, writes):
            return P.op("dve", lambda e: e.tensor_tensor_scan(out=out, data0=d0, data1=d1, initial=init, op0=ALU.mult, op1=ALU.add),
                        reads=reads, writes=writes)

        def rfast(ap, bufs):
            return P.op("dve", lambda e: e.reciprocal_approx_fast(out=ap, in_=ap), reads=bufs, writes=bufs)

        def recip(ap, bufs):
            return P.op("dve", lambda e: e.reciprocal(out=ap, in_=ap), reads=bufs, writes=bufs)

        tap_ops = []

        def tap(name, ap, reads):
            if name in tapd:
                tap_ops.append(P.op("sp", lambda e: e.dma_start(out=tapd[name].ap(), in_=ap), reads=reads, dma_key="tap_" + name))

        CH = 256
        conv_chunks = []
        r0 = 0
        while r0 < WROWS:
            r1 = min(WROWS, r0 + CH)
            conv_chunks.append((r0, r1))
            r0 = r1
        B_conv = [Buf("conv%d" % i) for i in range(len(conv_chunks))]
        conv_ops = []
        conv_gate = []

        def emit_conversions(lo, hi, gate=()):
          for i in range(lo, min(hi, len(conv_chunks))):
            a, b = conv_chunks[i]
            ed = [conv_ops[i - 2]] if i >= 2 else []
            if i == N_EARLY:
                ed = ed + conv_gate
            if i < lo + 2:
                ed = ed + list(gate)
            o = P.op("pool", (lambda a, b: lambda e: e.dma_start(out=wscr.ap()[a:b, :], in_=wflat.ap()[a:b, :]))(a, b),
                     writes=[B_conv[i]], dma_key="conv%d" % (i % 4), extra_deps=ed)
            conv_ops.append(o)

        N_EARLY = 3
        emit_conversions(0, N_EARLY)

        slot_ctr = [0]

        sticky_cache = {}

        def load_slab(name, sticky=False):
            if sticky and name in sticky_cache:
                return sticky_cache[name]
            r_ = load_slab_(name)
            if sticky:
                sticky_cache[name] = r_
            return r_

        def load_slab_(name):
            off, n = stab[name]
            s = slot_ctr[0] % NSLOT
            slot_ctr[0] += 1
            rlo = off // 2048
            rhi = (off + 128 * n - 1) // 2048
            cbufs = [B_conv[i] for i in range(rlo // CH, rhi // CH + 1)]
            src = bass.AP(wscr, off, [[n, 128], [1, n]])
            o_ = P.op("sp", lambda e: e.dma_start(out=ring[:, s, 0:n], in_=src), reads=cbufs, writes=[B_ring[s]], dma_key="ring%d" % s)
            if len(conv_ops) <= N_EARLY:
                conv_gate.append(o_)
            return s, B_ring[s]

        P.op("sp", lambda e: e.dma_start(out=vecs[:], in_=vecsd.ap()), writes=[B_vecs], dma_key="c_vecs")
        P.op("sp", lambda e: e.dma_start(out=ident[:], in_=identd.ap()), writes=[B_ident], dma_key="c_ident")
        P.op("sp", lambda e: e.dma_start(out=maskf[:], in_=maskd.ap()), writes=[B_mask], dma_key="c_mask")
        P.op("sp", lambda e: e.dma_start(out=sinkb[:], in_=bass.AP(sinkd, 0, [[0, 128], [1, 16]])), writes=[B_esink], dma_key="c_sink")
        P.op("sp", lambda e: e.dma_start(out=wr_f[:], in_=wrd.ap()), writes=[B_wr], dma_key="c_wr")
        P.op("sp", lambda e: e.dma_start(out=brt[:], in_=bass.AP(brd, 0, [[0, 128], [0, 4], [1, 8]])), writes=[B_brt], dma_key="c_br")

        P.op("dve", lambda e: e.memset(dummy[:], 1.0), writes=[B_dummy])
        P.op("dve", lambda e: e.memset(ones_f[:], 1.0), writes=[B_ones])
        P.op("dve", lambda e: e.memset(ones_b[:], 1.0), writes=[B_ones])
        P.op("dve", lambda e: e.memset(HS[:], 0.0), writes=B_HS)
        P.op("dve", lambda e: e.memset(XBh[:, :, 0:4], 0.0), writes=B_XBh)
        cp("dve", wr_b[:], wr_f[:], [B_wr], [B_wr])
        for c in range(8):
            for k in range(4):
                ts("dve", DG[:, c, k, :], ident[:], vcol("conv_w", 8 * k + c), ALU.mult, [B_ident, B_vecs], [B_DG])
        lam = vecs[:, VC["lam"]:VC["lam"] + 8]
        ts("dve", lruc[:, 3, :], lam, -1.0, ALU.mult, [B_vecs], [B_lruc])
        tt("dve", lruc[:, 2, :], lruc[:, 3, :], lam, ALU.max, [B_vecs, B_lruc], [B_lruc])
        act(lruc[:, 3, :], lruc[:, 2, :], AF.Exp, [B_lruc], [B_lruc], scale=-1.0)
        act(lruc[:, 2, :], lruc[:, 3, :], AF.Ln, [B_lruc], [B_lruc], bias=1.0)
        ts("dve", lruc[:, 3, :], lam, -1.0, ALU.mult, [B_vecs, B_lruc], [B_lruc], s2=0.0, op1=ALU.max)
        tt("dve", lruc[:, 3, :], lruc[:, 3, :], lruc[:, 2, :], ALU.add, [B_lruc], [B_lruc])
        ts("dve", lruc[:, 0, :], lruc[:, 3, :], -4.0, ALU.mult, [B_lruc], [B_lruc])
        ts("dve", lruc[:, 1, :], lruc[:, 3, :], -8.0, ALU.mult, [B_lruc], [B_lruc])
        cp("dve", ident_b[:], ident[:], [B_ident], [B_mask])
        for h in range(2):
            ts("dve", MASKB[:, 0, h * 128:(h + 1) * 128], maskf[:, 0, :], -1.0, ALU.add, [B_mask], [B_mask], s2=30000.0, op1=ALU.mult)
            ts("dve", MASKB[:, 0, 256 + h * 128:256 + (h + 1) * 128], maskf[:, 1, :], -1.0, ALU.add, [B_mask], [B_mask], s2=30000.0, op1=ALU.mult)
            ts("dve", MASKB[:, 1, 256 + h * 128:256 + (h + 1) * 128], maskf[:, 1, :], -1.0, ALU.add, [B_mask], [B_mask], s2=30000.0, op1=ALU.mult)
        ts("dve", maskf[:, 0, :], maskf[:, 0, :], vcol("carry"), ALU.mult, [B_mask, B_vecs], [B_mask])
        for h in range(2):
            ts("dve", MASKB[:, 1, h * 128:(h + 1) * 128], maskf[:, 0, :], -1.0, ALU.add, [B_mask], [B_mask], s2=30000.0, op1=ALU.mult)
        act(sinkb[:], sinkb[:], AF.Exp, [B_esink], [B_esink])
        for g in range(4):
            c0 = 2 * g
            for hh in range(2):
                ts("dve", ESINK2[0:64, g, hh * 128:(hh + 1) * 128], ones_f[0:64, :], sinkb[0:64, c0 + hh:c0 + hh + 1], ALU.mult, [B_ones, B_esink], [B_esink])
                ts("dve", ESINK2[64:128, g, hh * 128:(hh + 1) * 128], ones_f[64:128, :], sinkb[64:128, 8 + c0 + hh:9 + c0 + hh], ALU.mult, [B_ones, B_esink], [B_esink])

        last_x_load = [None]

        def issue_loads(ti):
            xb = ti % 2
            src = xT.ap().rearrange("(c p) t -> p c t", p=128)[:, :, ti * T:(ti + 1) * T]
            o_ = P.op("sp", lambda e: e.dma_start(out=X[xb][:], in_=src), writes=XB_X[xb], dma_key="x%d" % xb)
            last_x_load[0] = o_
            if len(conv_ops) <= N_EARLY:
                conv_gate.append(o_)
            if ti >= NPRE - 1:
                pj = ti - (NPRE - 1)
                psrc = pT.ap().rearrange("l (c p) t -> p l c t", p=128)[:, :, :, pj * T:(pj + 1) * T]
                P.op("sp", lambda e: e.dma_start(out=PST[xb][:], in_=psrc), writes=[B_PST[xb]], dma_key="p%d" % xb)
                possrc = bass.AP(posd, pj * T, [[0, 128], [1, T]])
                P.op("sp", lambda e: e.dma_start(out=POSI[xb][:], in_=possrc), writes=[B_POSI[xb]], dma_key="pos%d" % xb)

        def rmsnorm(xb, gname, final=False):
            Xt, BX = X[xb], XB_X[xb]
            bk, Bbk = nb()
            for c in range(8):
                if final:
                    sq, Bsq = bf16half(8 + c // 2, c % 2)
                else:
                    sq, Bsq = XN[:, c, :], [B_XN[c]]
                act(sq, Xt[:, c, :], AF.Square, [BX[c]], Bsq)
                mm(bk[:], ones_b[:], sq, c == 0, c == 7, [B_ones] + Bsq, [Bbk])
            rs, Brs = RS[:], [B_RS]
            act(rs, bk[:], AF.Ln, [Bbk], Brs, scale=1.0 / D, bias=EPS)
            act(rs, rs, AF.Exp, Brs, Brs, scale=-0.5)
            if final:
                for c in range(8):
                    tt("pool", Xt[:, c, :], Xt[:, c, :], rs, ALU.mult, [BX[c]] + Brs, [BX[c]])
                    ts("pool", Xt[:, c, :], Xt[:, c, :], vcol(gname, c), ALU.mult, [BX[c], B_vecs], [BX[c]], s2=1.0, op1=ALU.mult)
                return
            mm(bk[:], ident[:], rs, True, True, [B_ident] + Brs, [Bbk])
            for c in range(8):
                stt(XN[:, c, :], Xt[:, c, :], vcol(gname, c), bk[:], ALU.mult, ALU.mult, [BX[c], B_vecs, Bbk], [B_XN[c]])

        def presqrt():
            act(dummy[:, 0:1], dummy[:, 1:2], AF.Ln, [B_dummy], [B_dummy])

        def proj_add(xb, slab_names, src, Bsrc, kc_n=8):
            Xt, BX = X[xb], XB_X[xb]
            for h, nm in enumerate(slab_names):
                s, Bs = load_slab(nm)
                for mi in range(4):
                    m = 4 * h + mi
                    bk, Bbk = nb()
                    for kc in range(kc_n):
                        mm(W(bk), ring[:, s, kc * 512 + mi * 128: kc * 512 + (mi + 1) * 128], W(src(kc)),
                           kc == 0, kc == kc_n - 1, [Bs] + Bsrc(kc), [Bbk])
                    tt("dve", W(Xt[:, m, :]), W(bk), W(Xt[:, m, :]), ALU.add, [Bbk, BX[m]], [BX[m]])

        CW = [0, T]

        def W(ap):
            return ap[:, CW[0]:CW[1]]

        pre_normed = set()
        ALT_OK = (NPRE - 1 >= 1) and NSLOT >= 5
        B_alt = [Buf("alt%d" % j) for j in range(16)]

        def alt_view(j):
            if j < 8:
                sl, k = 3 + j // 4, j % 4
                return ring[:, sl, k * 1024:(k + 1) * 1024].bitcast(F32), [B_alt[j]]
            j2 = j - 8
            return PST[j2 // 4][:, (j2 % 4) // 2, j2 % 2, :], [B_alt[j]]

        def alt_fence():
            P.op("dve", lambda e: e.memset(dummy[:, 0:1], 1.0), reads=B_alt, writes=[B_ring[3], B_ring[4], B_PST[0], B_PST[1], B_dummy])

        def lru(xb, full, first_mode, normed=False, pool_ok=False):
            Xt, BX = X[xb], XB_X[xb]
            if not normed:
                rmsnorm(xb, "g_mix0")
            for h in range(2):
                s, Bs = load_slab("in_x%d" % h, sticky=not full)
                for mi in range(4):
                    c = 4 * h + mi
                    bk, Bbk = nb()
                    for kc in range(8):
                        mm(bk[:], ring[:, s, kc * 512 + mi * 128: kc * 512 + (mi + 1) * 128], XN[:, kc, :],
                           kc == 0, kc == 7, [Bs, B_XN[kc]], [Bbk])
                    if pool_ok:
                        act(XBh[:, c, 4:4 + T], bk[:], AF.Copy, [Bbk], [B_XBh[c]])
                    else:
                        cp("dve", XBh[:, c, 4:4 + T], bk[:], [Bbk], [B_XBh[c]])
            GATEv = [bf16half(c // 2, c % 2) for c in range(8)]
            def gate_branch():
                for h in range(2):
                    s, Bs = load_slab("in_g%d" % h)
                    for mi in range(4):
                        c = 4 * h + mi
                        bk, Bbk = nb()
                        for kc in range(8):
                            mm(W(bk), ring[:, s, kc * 512 + mi * 128: kc * 512 + (mi + 1) * 128], W(XN[:, kc, :]),
                               kc == 0, kc == 7, [Bs, B_XN[kc]], [Bbk])
                        act(W(GATEv[c][0]), W(bk), AF.Gelu_apprx_tanh, [Bbk], GATEv[c][1])
            sg, Bsg = load_slab("gates", sticky=not full)
            XCbv = [bf16half(4 + c // 2, c % 2) for c in range(8)]
            for half in range(2):
                cs = [4 * half + i for i in range(4)]
                if half == 1 and not full and ALT_OK:
                    XCf = {c: alt_view(0 + i) for i, c in enumerate(cs)}
                    Ip = {c: alt_view(4 + i) for i, c in enumerate(cs)}
                    Av = {c: alt_view(8 + i) for i, c in enumerate(cs)}
                    Mv = {c: alt_view(12 + i) for i, c in enumerate(cs)}
                else:
                    XCf = {c: f32pg(8 + i) for i, c in enumerate(cs)}
                    Ip = {c: f32pg(12 + i) for i, c in enumerate(cs)}
                    Av = {c: f32pg(16 + i) for i, c in enumerate(cs)}
                    Mv = {c: f32pg(20 + i) for i, c in enumerate(cs)}
                for c in cs:
                    bk, Bbk = nb()
                    for k in range(4):
                        mm(bk[:], DG[:, c, k, :], XBh[:, c, 4 - k:4 - k + T], k == 0, k == 3, [B_DG, B_XBh[c]], [Bbk])
                    act(XCf[c][0], bk[:], AF.Identity, [Bbk, B_vecs], XCf[c][1], bias=vcol("conv_b", c))
                    if pool_ok:
                        cp("pool", XCbv[c][0], XCf[c][0], XCf[c][1], XCbv[c][1])
                    else:
                        cp("dve", XCbv[c][0], XCf[c][0], XCf[c][1], XCbv[c][1])
                    cp("dve", XBh[:, c, 1:4], XBh[:, c, T + 1:T + 4], [B_XBh[c]], [B_XBh[c]])
                Rp = {}
                for oc in cs:
                    hb, jh = oc // 2, oc % 2
                    for g in range(2):
                        bk, Bbk = nb()
                        for i in range(2):
                            base = ((g * 4 + hb) * 2 + i) * 256 + jh * 128
                            mm(bk[:], ring[:, sg, base:base + 128], XCbv[2 * hb + i][0], i == 0, i == 1,
                               [Bsg] + XCbv[2 * hb + i][1], [Bbk])
                        if g == 0:
                            act(Mv[oc][0], bk[:], AF.Tanh, [Bbk, B_lruc], Mv[oc][1], scale=0.5, bias=hbias(0, oc))
                        else:
                            act(Ip[oc][0], bk[:], AF.Tanh, [Bbk, B_lruc], Ip[oc][1], scale=0.5, bias=hbias(1, oc))
                for c in cs:
                    act(Av[c][0], Mv[c][0], AF.Exp, Mv[c][1] + [B_lruc], Av[c][1], scale=lruc[:, 0, c:c + 1], bias=lruc[:, 0, c:c + 1])
                for c in cs:
                    if pool_ok:
                        tt("pool", Mv[c][0], Av[c][0], Av[c][0], ALU.mult, Av[c][1], Mv[c][1])
                    else:
                        act(Mv[c][0], Mv[c][0], AF.Exp, Mv[c][1] + [B_lruc], Mv[c][1], scale=lruc[:, 1, c:c + 1], bias=lruc[:, 1, c:c + 1])
                for c in cs:
                    act(Mv[c][0], Mv[c][0], AF.Ln, Mv[c][1], Mv[c][1], scale=-1.0, bias=1.0)
                for c in cs:
                    act(Mv[c][0], Mv[c][0], AF.Exp, Mv[c][1], Mv[c][1], scale=0.5)
                if half == 0 and full:
                    gate_branch()
                for c in cs:
                    if first_mode == "one":
                        memset("dve", Mv[c][0][:, 0:1], 1.0, Mv[c][1])
                    elif first_mode == "blend":
                        ts("dve", Mv[c][0][:, 0:1], Mv[c][0][:, 0:1], vcol("carry"), ALU.mult, Mv[c][1] + [B_vecs], Mv[c][1],
                           s2=vcol("omc"), op1=ALU.add)
                        ts("dve", HS[:, c:c + 1], HS[:, c:c + 1], vcol("carry"), ALU.mult, [B_HS[c], B_vecs], [B_HS[c]])
                    stt(Ip[c][0], Ip[c][0], 1.0, XCf[c][0], ALU.add, ALU.mult, Ip[c][1] + XCf[c][1], Ip[c][1])
                    stt(Ip[c][0], Ip[c][0], 0.5, Mv[c][0], ALU.mult, ALU.mult, Ip[c][1] + Mv[c][1], Ip[c][1])
                    scan(XCf[c][0], Av[c][0], Ip[c][0], HS[:, c:c + 1], Av[c][1] + Ip[c][1] + [B_HS[c]], XCf[c][1])
                    cp("dve", HS[:, c:c + 1], XCf[c][0][:, T - 1:T], XCf[c][1], [B_HS[c]])
                    if full:
                        tt("pool" if pool_ok else "dve", W(XBh[:, c, 4:4 + T]), W(XCf[c][0]), W(GATEv[c][0]), ALU.mult, XCf[c][1] + GATEv[c][1], [B_XBh[c]])
            if full:
                proj_add(xb, ["out0", "out1"], lambda kc: XBh[:, kc, 4:4 + T], lambda kc: [B_XBh[kc]])

        def hbias(g, oc):
            return hb_tab[:, g, oc:oc + 1]

        hb_tab = sb("hb_tab", [128, 2, 8], F32)
        ts("dve", hb_tab[:, 0, :], vecs[:, VC["b_ga"]:VC["b_ga"] + 8], 0.5, ALU.mult, [B_vecs], [B_lruc])
        ts("dve", hb_tab[:, 1, :], vecs[:, VC["b_gx"]:VC["b_gx"] + 8], 0.5, ALU.mult, [B_vecs], [B_lruc])

        def ffn(xb):
            rmsnorm(xb, "g_ffn0")
            HID = [bf16half(j // 2, j % 2) for j in range(24)]
            for s12 in range(12):
                s, Bs = load_slab("ffin%d" % s12)
                for jj in range(2):
                    j = 2 * s12 + jj
                    bg, Bbg = nb()
                    bu, Bbu = nb()
                    for kc in range(8):
                        mm(W(bg), ring[:, s, kc * 512 + jj * 128: kc * 512 + (jj + 1) * 128], W(XN[:, kc, :]), kc == 0, kc == 7, [Bs, B_XN[kc]], [Bbg])
                    for kc in range(8):
                        mm(W(bu), ring[:, s, kc * 512 + (2 + jj) * 128: kc * 512 + (3 + jj) * 128], W(XN[:, kc, :]), kc == 0, kc == 7, [Bs, B_XN[kc]], [Bbu])
                    sgv, Bsgv = f32pg(12 + (j % 4))
                    act(W(sgv), W(bg), AF.Silu, [Bbg], Bsgv)
                    tt("dve", W(HID[j][0]), W(bu), W(sgv), ALU.mult, [Bbu] + Bsgv, HID[j][1])
                if s12 == 5:
                    rope_sin()
            presqrt()
            Xt, BX = X[xb], XB_X[xb]
            for m in range(8):
                s, Bs = load_slab("ffout%d" % m)
                bk, Bbk = nb()
                for j in range(24):
                    mm(W(bk), ring[:, s, j * 128:(j + 1) * 128], W(HID[j][0]), j == 0, j == 23, [Bs] + HID[j][1], [Bbk])
                tt("dve", W(Xt[:, m, :]), W(bk), W(Xt[:, m, :]), ALU.add, [Bbk, BX[m]], [BX[m]])

        def ple(xb, layer):
            Xt, BX = X[xb], XB_X[xb]
            rmsnorm(xb, "g_ple%d" % layer)
            cp("dve", PT[:], PST[xb][:, layer, :, :], [B_PST[xb]], [B_PT])
            GP = [f32pg(m) for m in range(8)]
            for h in range(2):
                s, Bs = load_slab("pleg%d_%d" % (layer, h))
                for mi in range(4):
                    m = 4 * h + mi
                    bk, Bbk = nb()
                    for kc in range(8):
                        mm(W(bk), ring[:, s, kc * 512 + mi * 128: kc * 512 + (mi + 1) * 128], W(XN[:, kc, :]), kc == 0, kc == 7, [Bs, B_XN[kc]], [Bbk])
                    act(W(GP[m][0]), W(bk), AF.Sigmoid, [Bbk], GP[m][1])
            presqrt()
            s, Bs = load_slab("plep%d" % layer)
            for m in range(8):
                bk, Bbk = nb()
                for kc in range(2):
                    mm(W(bk), ring[:, s, kc * 1024 + m * 128: kc * 1024 + (m + 1) * 128], W(PT[:, kc, :]), kc == 0, kc == 1, [Bs, B_PT], [Bbk])
                tt("dve", W(GP[m][0]), W(bk), W(GP[m][0]), ALU.mult, [Bbk] + GP[m][1], GP[m][1])
                tt("dve", W(Xt[:, m, :]), W(GP[m][0]), W(Xt[:, m, :]), ALU.add, GP[m][1] + [BX[m]], [BX[m]])

        def rope_tables(xb):
            a0, a1, a2 = RT[0], RT[1], RT[2]
            Ba = B_RT
            cp("dve", a0[:], POSI[xb][:], [B_POSI[xb]], [Ba[0]])
            ts("dve", a0[:], a0[:], vcol("freq"), ALU.mult, [Ba[0], B_vecs], [Ba[0]])
            ts("dve", RTI[:], a0[:], 1.0 / (2 * PI), ALU.mult, [Ba[0]], [B_RTI])
            cp("dve", a1[:], RTI[:], [B_RTI], [Ba[1]])
            C1 = 6.28125
            C2 = float(2 * np.pi - 6.28125)
            stt(a0[:], a1[:], -C1, a0[:], ALU.mult, ALU.add, [Ba[1], Ba[0]], [Ba[0]])
            stt(a0[:], a1[:], -C2, a0[:], ALU.mult, ALU.add, [Ba[1], Ba[0]], [Ba[0]])
            ts("dve", a0[:], a0[:], PI, ALU.min, [Ba[0]], [Ba[0]], s2=-PI, op1=ALU.max)
            ts("dve", a1[:], a0[:], PI / 2, ALU.is_gt, [Ba[0]], [Ba[1]])
            stt(a1[:], a1[:], -2 * PI, a0[:], ALU.mult, ALU.add, [Ba[1], Ba[0]], [Ba[1]])
            ts("dve", a1[:], a1[:], PI / 2, ALU.add, [Ba[1]], [Ba[1]], s2=PI, op1=ALU.min)

        def rope_sin():
            act(SIN[:], RT[0][:], AF.Sin, [B_RT[0], B_vecs], [B_CS], scale=vcol("sign"))
            act(COS[:], RT[1][:], AF.Sin, [B_RT[1]], [B_CS])

        def kv(xb):
            rmsnorm(xb, "g_kv")
            s, Bs = load_slab("kv")
            bk, Bbk = nb()
            bk2, Bbk2 = nb()
            for kc in range(8):
                mm(W(bk), ring[:, s, kc * 384: kc * 384 + 128], W(XN[:, kc, :]), kc == 0, kc == 7, [Bs, B_XN[kc]], [Bbk])
            for kc in range(8):
                mm(W(bk2), ring[:, s, kc * 384 + 128: kc * 384 + 256], W(XN[:, kc, :]), kc == 0, kc == 7, [Bs, B_XN[kc]], [Bbk2])
            t1, Bt1 = f32pg(0)
            t2, Bt2 = f32pg(1)
            tt("dve", W(t1), W(bk), W(COS), ALU.mult, [Bbk, B_CS], Bt1)
            tt("dve", W(t2), W(bk2), W(SIN), ALU.mult, [Bbk2, B_CS], Bt2)
            tt("dve", KT[:, 128 + CW[0]:128 + CW[1]], W(t1), W(t2), ALU.add, Bt1 + Bt2, B_KT[1:5])
            bv, Bbv = nb()
            b0 = CW[0] // 128
            for blk in range(b0, 4):
                for kc in range(8):
                    mm(bv[:, blk * 128:(blk + 1) * 128], XN[:, kc, blk * 128:(blk + 1) * 128], ring[:, s, kc * 384 + 256: kc * 384 + 384],
                       kc == 0, kc == 7, [Bs, B_XN[kc]], [Bbv])
            act(VT[:, 1 + b0:5, :], bv[:, b0 * 128:512].rearrange("p (b f) -> p b f", b=4 - b0), AF.Copy, [Bbv], B_VT[1:5])

        def kv_shift():
            cp("dve", KT[:, 0:128], KT[:, 512:640], [B_KT[4]], [B_KT[0]])
            cp("dve", VT[:, 0, :], VT[:, 4, :], [B_VT[4]], [B_VT[0]])

        def attention(xb, first_real):
            Xt, BX = X[xb], XB_X[xb]
            rmsnorm(xb, "g_mix1")
            QR = [bf16half(4 + c // 2, c % 2) for c in range(8)]
            for s4 in range(4):
                s, Bs = load_slab("q%d" % s4)
                for ci in range(2):
                    c = 2 * s4 + ci
                    bq, Bbq = nb()
                    bq2, Bbq2 = nb()
                    for kc in range(8):
                        mm(bq[:], ring[:, s, kc * 512 + (2 * ci) * 128: kc * 512 + (2 * ci + 1) * 128], XN[:, kc, :], kc == 0, kc == 7, [Bs, B_XN[kc]], [Bbq])
                    for kc in range(8):
                        mm(bq2[:], ring[:, s, kc * 512 + (2 * ci + 1) * 128: kc * 512 + (2 * ci + 2) * 128], XN[:, kc, :], kc == 0, kc == 7, [Bs, B_XN[kc]], [Bbq2])
                    t1, Bt1 = f32pg(c % 2)
                    t2, Bt2 = f32pg(2 + c % 2)
                    tt("dve", t1, bq[:], COS[:], ALU.mult, [Bbq, B_CS], Bt1)
                    tt("dve", t2, bq2[:], SIN[:], ALU.mult, [Bbq2, B_CS], Bt2)
                    tt("pool", QR[c][0], t1, t2, ALU.add, Bt1 + Bt2, QR[c][1])
            ATT = [bf16half(8 + c // 2, c % 2) for c in range(8)]
            ATTall = pview(8, BF16, 8 * T).rearrange("p (c t) -> p c t", c=8)
            gi = 0
            for qb in range(4):
                for g in range(4):
                    c0 = 2 * g
                    par = gi % 2
                    gi += 1
                    EA, BEA = bf16half(12 + par, 0)
                    EB, BEB = bf16half(12 + par, 1)
                    sa, Bsa = nb()
                    sbk, Bsb = nb()
                    zu, Bzu = nb()
                    kprev = KT[:, qb * 128:(qb + 1) * 128]
                    kcur = KT[:, (qb + 1) * 128:(qb + 2) * 128]
                    Bk = [B_KT[qb], B_KT[qb + 1]]
                    mkb = MASKB[:, 1 if (first_real and qb == 0) else 0, :]
                    for base, sbank, Bsbank in ((0, sa, Bsa), (64, sbk, Bsb)):
                        mm(sbank[:], ident_b[:], mkb, True, False, [B_mask], [Bsbank])
                        for jc, ksl in enumerate((kprev, kcur)):
                            for hh in range(2):
                                c = c0 + hh
                                col = (2 * jc + hh) * 128
                                mm(sbank[:, col:col + 128], ksl[base:base + 64, :], QR[c][0][base:base + 64, qb * 128:(qb + 1) * 128],
                                   False, True, Bk + QR[c][1], [Bsbank])
                    act(EA, sa[:], AF.Exp, [Bsa], BEA, scale=0.125)
                    act(EB, sbk[:], AF.Exp, [Bsb], BEB, scale=0.125)
                    Bv = [B_VT[qb], B_VT[qb + 1]]
                    for bi, (Ev, BEv) in enumerate(((EA, BEA), (EB, BEB))):
                        r0 = 64 * bi
                        mm(zu[r0:r0 + 64, 256:512], ones_b[:, 0:64], Ev[:, 0:256], True, False, [B_ones] + BEv, [Bzu])
                        mm(zu[r0:r0 + 64, 256:512], ones_b[:, 0:64], Ev[:, 256:512], False, True, [B_ones] + BEv, [Bzu])
                        for hh in range(2):
                            mm(zu[r0:r0 + 64, hh * 128:(hh + 1) * 128], VT[:, qb, r0:r0 + 64], Ev[:, hh * 128:(hh + 1) * 128], True, False, Bv + BEv, [Bzu])
                            mm(zu[r0:r0 + 64, hh * 128:(hh + 1) * 128], VT[:, qb + 1, r0:r0 + 64], Ev[:, 256 + hh * 128:256 + (hh + 1) * 128], False, True, Bv + BEv, [Bzu])
                    rz = pview(16 + par, F32, 256)
                    Brz = [B_pg[16 + par]]
                    tt("dve", rz, zu[:, 256:512], ESINK2[:, g, :], ALU.add, [Bzu, B_esink], Brz)
                    act(rz, rz, AF.Ln, Brz, Brz)
                    act(rz, rz, AF.Exp, Brz, Brz, scale=-1.0)
                    oall = ATTall[:, c0:c0 + 2, qb * 128:(qb + 1) * 128]
                    Batt = ATT[c0][1]
                    tt("dve", oall, zu[:, 0:256].rearrange("p (h q) -> p h q", h=2), rz.rearrange("p (h q) -> p h q", h=2),
                       ALU.mult, [Bzu] + Brz, Batt)
            presqrt()
            proj_add(xb, ["o0", "o1"], lambda kc: ATT[kc][0], lambda kc: ATT[kc][1])

        def moe(xb):
            Xt, BX = X[xb], XB_X[xb]
            rmsnorm(xb, "g_ffn1")
            bl, Bbl = nb()
            for blk in range(4):
                for kc in range(8):
                    mm(bl[:, blk * 8:(blk + 1) * 8], XN[:, kc, blk * 128:(blk + 1) * 128], wr_b[:, kc * 8:(kc + 1) * 8], kc == 0, kc == 7,
                       [B_XN[kc], B_wr], [Bbl])
            Bs_ = [B_rt_small]
            tt("dve", RL[:], bl[:, 0:32].rearrange("p (b e) -> p b e", b=4), brt[:], ALU.add, [Bbl, B_brt], Bs_)
            P.op("dve", lambda e: e.tensor_reduce(out=RM[:, :, 0], in_=RL[:], axis=AX.X, op=ALU.max), reads=Bs_, writes=Bs_)
            m1b = bass.AP(RM, 0, [[16, 128], [4, 4], [0, 8]])
            m2b = bass.AP(RM, 1, [[16, 128], [4, 4], [0, 8]])
            w1b = bass.AP(RM, 2, [[16, 128], [4, 4], [0, 8]])
            w2b = bass.AP(RM, 3, [[16, 128], [4, 4], [0, 8]])
            tt("dve", REQ1[:], RL[:], m1b, ALU.is_equal, Bs_, Bs_)
            stt(RL2[:], REQ1[:], -1e30, RL[:], ALU.mult, ALU.add, Bs_, Bs_)
            P.op("dve", lambda e: e.tensor_reduce(out=RM[:, :, 1], in_=RL2[:], axis=AX.X, op=ALU.max), reads=Bs_, writes=Bs_)
            tt("dve", REQ2[:], RL2[:], m2b, ALU.is_equal, Bs_, Bs_)
            tt("dve", RM[:, :, 2], RM[:, :, 0], RM[:, :, 1], ALU.subtract, Bs_, Bs_)
            act(RM[:, :, 3], RM[:, :, 2], AF.Tanh, Bs_, Bs_, scale=0.5)
            ts("dve", RM[:, :, 2], RM[:, :, 3], 0.5, ALU.mult, Bs_, Bs_, s2=0.5, op1=ALU.add)
            ts("dve", RM[:, :, 3], RM[:, :, 3], -0.5, ALU.mult, Bs_, Bs_, s2=0.5, op1=ALU.add)
            tt("dve", REQ1[:], REQ1[:], w1b, ALU.mult, Bs_, Bs_)
            tt("dve", REQ2[:], REQ2[:], w2b, ALU.mult, Bs_, Bs_)
            tt("dve", RGT[:], REQ1[:], REQ2[:], ALU.add, Bs_, Bs_)
            GE = [f32pg(e) for e in range(8)]
            identb = bass.AP(ident, 0, [[128, 128], [0, 4], [1, 128]])
            for e in range(8):
                de, Bde = DE[e % 2], B_DE[e % 2]
                gsrc = bass.AP(RGT, e, [[32, 128], [8, 4], [0, 128]])
                tt("dve", de[:], identb, gsrc, ALU.mult, [B_ident] + Bs_, [Bde])
                bk, Bbk = nb()
                mm(bk[:], ones_f[:], de[:].rearrange("p b t -> p (b t)"), True, True, [B_ones, Bde], [Bbk])
                act(GE[e][0], bk[:], AF.Copy, [Bbk], GE[e][1])
            for e in range(8):
                HE = [bf16half(8 + 4 * (e % 2) + j // 2, j % 2) for j in range(8)]
                for s4 in range(4):
                    s, Bs = load_slab("ein%d_%d" % (e, s4))
                    for jj in range(2):
                        j = 2 * s4 + jj
                        bg, Bbg = nb()
                        bu, Bbu = nb()
                        for kc in range(8):
                            mm(bg[:], ring[:, s, kc * 512 + jj * 128: kc * 512 + (jj + 1) * 128], XN[:, kc, :], kc == 0, kc == 7, [Bs, B_XN[kc]], [Bbg])
                        for kc in range(8):
                            mm(bu[:], ring[:, s, kc * 512 + (2 + jj) * 128: kc * 512 + (3 + jj) * 128], XN[:, kc, :], kc == 0, kc == 7, [Bs, B_XN[kc]], [Bbu])
                        sgv, Bsgv = f32pg(16 + (j % 4))
                        sgg, Bsgg = f32pg(20 + (j % 4))
                        act(sgv, bg[:], AF.Silu, [Bbg], Bsgv)
                        tt("pool", sgg, sgv, GE[e][0], ALU.mult, Bsgv + GE[e][1], Bsgg)
                        tt("dve", HE[j][0], bu[:], sgg, ALU.mult, [Bbu] + Bsgg, HE[j][1])
                if e == 7:
                    presqrt()
                proj_add(xb, ["eout%d_0" % e, "eout%d_1" % e], lambda kc: HE[kc][0], lambda kc: HE[kc][1])

        CONV_PER_PRE = 9
        issue_loads(0)
        out_ops = []
        for ti in range(NT_ALL):
            xb = ti % 2
            is_pre = ti < NPRE - 1
            is_halo = ti == NPRE - 1
            ri = ti - NPRE
            first_mode = "one" if ti == 0 else ("blend" if ri == 0 else None)
            if is_pre:
                lru(xb, False, first_mode)
                if ti == NPRE - 2 and ALT_OK:
                    alt_fence()
                if ti + 1 < NT_ALL:
                    issue_loads(ti + 1)
                lo = N_EARLY + ti * CONV_PER_PRE
                hi = len(conv_chunks) if ti == NPRE - 2 else lo + CONV_PER_PRE
                emit_conversions(lo, hi, gate=[last_x_load[0]])
                continue
            if ri >= 0:
                kv_shift()
            CW[0] = (T - 128) if is_halo else 0
            rope_tables(xb)
            lru(xb, True, first_mode, normed=(ti in pre_normed), pool_ok=(ri >= 1))
            if ri == 0:
                tap("x_lru", X[xb][:], XB_X[xb])
            if ti + 1 < NT_ALL:
                issue_loads(ti + 1)
            ffn(xb)
            if ri == 0:
                tap("x_ffn", X[xb][:], XB_X[xb])
            ple(xb, 0)
            if ri == 0:
                tap("x_ple0", X[xb][:], XB_X[xb])
            kv(xb)
            if is_halo:
                continue
            attention(xb, ri == 0)
            if ri == 0:
                tap("x_att", X[xb][:], XB_X[xb])
            moe(xb)
            if ri == 0:
                tap("x_moe", X[xb][:], XB_X[xb])
            ple(xb, 1)
            if ti + 1 < NT_ALL:
                rmsnorm((ti + 1) % 2, "g_mix0")
                pre_normed.add(ti + 1)
            rmsnorm(xb, "g_final", final=True)
            dst = outT.ap().rearrange("(c p) t -> p c t", p=128)[:, :, ri * T:(ri + 1) * T]
            out_ops.append(P.op("pool", (lambda dst, xb: lambda e: e.dma_start(out=dst, in_=X[xb][:]))(dst, xb),
                                reads=XB_X[xb], dma_key="out%d" % xb))
        P.emit(final_waits=out_ops + tap_ops)
    return nc


_CACHE = {}


def _consts():
    ident = np.eye(128, dtype=np.float32)
    j = np.arange(128)[:, None]
    q = np.arange(128)[None, :]
    mask = np.zeros((128, 2, 128), np.float32)
    mask[:, 0, :] = (j > q)
    mask[:, 1, :] = (j <= q)
    return ident, mask


def run(inputs, n_cores, taps=(), trace=False):
    x = np.asarray(inputs["x"], np.float32)
    B, S, _ = x.shape
    HALF = S // 2
    assert B * 2 == n_cores and HALF % T == 0
    NPRE = NREAL = HALF // T
    p = np.asarray(inputs["p"], np.float32)
    pos = np.asarray(inputs["positions"], np.int32)
    slabs = build_slabs({k: np.asarray(v) for k, v in inputs.items()})
    wflat = np.concatenate([a.reshape(-1) for _, a in slabs]).astype(np.float32).reshape(-1, 2048)
    stab, wtotal = slab_table()
    assert wflat.size == wtotal
    ident, mask = _consts()
    wr = np.ascontiguousarray(np.asarray(inputs["w_router"][0], np.float32).reshape(8, 128, 8).transpose(1, 0, 2).reshape(128, 64))
    br = np.asarray(inputs["b_router"][0], np.float32).reshape(1, 8)
    sinks = np.asarray(inputs["attn_sinks"][0], np.float32).reshape(1, 16)
    key = (NPRE, NREAL, tuple(taps))
    if key not in _CACHE:
        _CACHE[key] = build_program(NPRE, NREAL, taps)
    nc = _CACHE[key]
    in_maps = []
    for cid in range(n_cores):
        b, half = cid // 2, cid % 2
        xT = np.zeros((D, 2 * HALF), np.float32)
        pTc = np.zeros((2, 256, T + HALF), np.float32)
        posc = np.zeros((1, T + HALF), np.int32)
        if half == 0:
            xT[:, HALF:] = x[b, 0:HALF].T
            pTc[:, :, T:] = p[:, b, 0:HALF].transpose(0, 2, 1)
            posc[0, T:] = pos[b, 0:HALF]
        else:
            xT[:] = x[b].T
            pTc[:] = p[:, b, HALF - T:S].transpose(0, 2, 1)
            posc[0] = pos[b, HALF - T:S]
        in_maps.append({
            "xT": xT, "pT": pTc, "pos": posc, "wflat": wflat, "vecs": build_vecs(inputs, float(half)),
            "ident": ident, "maskc": mask, "sinks": sinks, "wr": wr, "br": br,
        })
    res = run_bass_kernel_spmd(nc, in_maps, core_ids=list(range(n_cores)), trace=trace)
    out = np.zeros((B, S, D), np.float32)
    for cid in range(n_cores):
        b, half = cid // 2, cid % 2
        out[b, half * HALF:(half + 1) * HALF] = res.results[cid]["outT"].T
    return out, res


def kernel(**inputs):
    out, _ = run(inputs, 8)
    return out
```

```python
import contextlib
import numpy as np
import concourse.bass as bass
import concourse.mybir as mybir
from concourse.bass_utils import run_bass_kernel_spmd

F32 = mybir.dt.float32
BF16 = mybir.dt.bfloat16
I32 = mybir.dt.int32
U8 = mybir.dt.uint8
AF = mybir.ActivationFunctionType
ALU = mybir.AluOpType
AX = mybir.AxisListType

ENGS = ("pe", "act", "dve", "pool", "sp")
T = 512
D = 1024
NC8 = 8
EPS = 1e-6
NSLOT = 5
SLOT_ELEMS = 4096
PI = float(np.pi)
import os
PSUM_NORM = os.environ.get("K_PSUM_NORM", "0") == "1"


class Buf:
    __slots__ = ("name", "w", "r")

    def __init__(self, name):
        self.name = name
        self.w = None
        self.r = {}


class Op:
    __slots__ = ("eng", "fn", "deps", "signal", "count", "dma_sem", "dma_count")

    def __init__(self, eng, fn):
        self.eng = eng
        self.fn = fn
        self.deps = []
        self.signal = False
        self.count = 0
        self.dma_sem = None
        self.dma_count = 0


class Prog:
    def __init__(self, nc):
        self.nc = nc
        self.ops = {e: [] for e in ENGS}
        self.dma_sems = {}
        self.all_ops = []

    def op(self, eng, fn, reads=(), writes=(), dma_key=None, extra_deps=()):
        o = Op(eng, fn)
        deps = []
        for b in reads:
            if b.w is not None:
                deps.append((b.w, True))
        for b in writes:
            if b.w is not None:
                deps.append((b.w, False))
            for r in b.r.values():
                deps.append((r, False))
        for d in extra_deps:
            deps.append((d, True))
        seen = set()
        for d, raw in deps:
            if d is o or id(d) in seen:
                continue
            if d.dma_sem is None and d.eng == eng and (not raw or eng in ("pe", "sp")):
                continue
            seen.add(id(d))
            o.deps.append(d)
        is_dma = dma_key is not None
        for b in reads:
            b.r[(eng, id(o)) if is_dma else eng] = o
        for b in writes:
            b.w = o
            b.r = {}
        if is_dma:
            ent = self.dma_sems.setdefault(dma_key, [len(self.dma_sems), 0])
            ent[1] += 16
            o.dma_sem = ent[0]
            o.dma_count = ent[1]
        self.ops[eng].append(o)
        self.all_ops.append(o)
        return o

    def emit(self, final_waits=()):
        nc = self.nc
        for o in self.all_ops:
            for d in o.deps:
                if d.dma_sem is None:
                    d.signal = True
        for o in final_waits:
            if o.dma_sem is None:
                o.signal = True
        for e in ENGS:
            c = 0
            for o in self.ops[e]:
                if o.signal:
                    c += 1
                    o.count = c
        with contextlib.ExitStack() as st:
            esem = {e: st.enter_context(nc.semaphore("s_" + e)) for e in ENGS}
            dsem = [st.enter_context(nc.semaphore("d_%d" % i)) for i in range(len(self.dma_sems))]
            block = st.enter_context(nc.Block())

            def gen(ename):
                def body(eng):
                    waited = {}
                    dwaited = {}
                    for o in self.ops[ename]:
                        for d in o.deps:
                            if d.dma_sem is not None:
                                if dwaited.get(d.dma_sem, 0) < d.dma_count:
                                    eng.wait_ge(dsem[d.dma_sem], d.dma_count)
                                    dwaited[d.dma_sem] = d.dma_count
                            else:
                                if waited.get(d.eng, 0) < d.count:
                                    eng.wait_ge(esem[d.eng], d.count)
                                    waited[d.eng] = d.count
                        ins = o.fn(eng)
                        if o.dma_sem is not None:
                            ins.then_inc(dsem[o.dma_sem], 16)
                        elif o.signal:
                            ins.then_inc(esem[ename], 1)
                    if ename == "sp":
                        for o in final_waits:
                            if o.dma_sem is not None:
                                eng.wait_ge(dsem[o.dma_sem], o.dma_count)
                            else:
                                eng.wait_ge(esem[o.eng], o.count)
                return body

            block.tensor(gen("pe"))
            block.scalar(gen("act"))
            block.vector(gen("dve"))
            block.gpsimd(gen("pool"))
            block.sync(gen("sp"))


def _qcols():
    cols = np.zeros((8, 128), np.int64)
    for c in range(8):
        for p in range(128):
            head = c if p < 64 else 8 + c
            cols[c, p] = head * 64 + (p % 64)
    return cols


def _ropeswap(cols):
    out = cols.copy()
    d = cols % 64
    out = np.where(d < 8, cols + 8, np.where(d < 16, cols - 8, cols))
    return out


def _slab(W, cols):
    K = W.shape[0]
    kc = K // 128
    sub = W[:, cols]
    n = sub.shape[1]
    return np.ascontiguousarray(sub.reshape(kc, 128, n).transpose(1, 0, 2).reshape(128, kc * n))


def build_slabs(inp):
    S = []
    r = np.arange
    w_in = inp["w_lru_in"][0]
    for h in range(2):
        S.append(("in_x%d" % h, _slab(w_in, 1024 + h * 512 + r(512))))
    g = np.stack([inp["w_lru_ga"][0], inp["w_lru_gx"][0]])
    g = g.reshape(2, 4, 2, 128, 256).transpose(3, 0, 1, 2, 4).reshape(128, 4096)
    S.append(("gates", np.ascontiguousarray(g)))
    for h in range(2):
        S.append(("in_g%d" % h, _slab(w_in, h * 512 + r(512))))
    for h in range(2):
        S.append(("out%d" % h, _slab(inp["w_lru_out"][0], h * 512 + r(512))))
    wf = inp["w_ff_in"][0]
    for s in range(12):
        j = 2 * s
        cols = np.concatenate([j * 128 + r(128), (j + 1) * 128 + r(128), 3072 + j * 128 + r(128), 3072 + (j + 1) * 128 + r(128)])
        S.append(("ffin%d" % s, _slab(wf, cols)))
    for m in range(8):
        S.append(("ffout%d" % m, _slab(inp["w_ff_out"][0], m * 128 + r(128))))
    for h in range(2):
        S.append(("pleg0_%d" % h, _slab(inp["w_ple_gate"][0], h * 512 + r(512))))
    S.append(("plep0", _slab(inp["w_ple_proj"][0], r(1024))))
    kcols = r(128)
    S.append(("kv", _slab(inp["w_kv"], np.concatenate([kcols, _ropeswap(kcols), 128 + r(128)]))))
    qc = _qcols()
    wq = inp["w_q"][0]
    for s in range(4):
        c0, c1 = 2 * s, 2 * s + 1
        cols = np.concatenate([qc[c0], _ropeswap(qc[c0]), qc[c1], _ropeswap(qc[c1])])
        S.append(("q%d" % s, _slab(wq, cols)))
    wo = inp["w_o"][0][qc.reshape(-1), :]
    for h in range(2):
        S.append(("o%d" % h, _slab(wo, h * 512 + r(512))))
    for e in range(8):
        we = inp["w_exp_in"][0][e]
        for s in range(4):
            j = 2 * s
            cols = np.concatenate([j * 128 + r(128), (j + 1) * 128 + r(128), 1024 + j * 128 + r(128), 1024 + (j + 1) * 128 + r(128)])
            S.append(("ein%d_%d" % (e, s), _slab(we, cols)))
        for h in range(2):
            S.append(("eout%d_%d" % (e, h), _slab(inp["w_exp_out"][0][e], h * 512 + r(512))))
    for h in range(2):
        S.append(("pleg1_%d" % h, _slab(inp["w_ple_gate"][1], h * 512 + r(512))))
    S.append(("plep1", _slab(inp["w_ple_proj"][1], r(1024))))
    return S


def slab_table():
    names = []
    for h in range(2):
        names.append(("in_x%d" % h, 4096))
    names.append(("gates", 4096))
    for h in range(2):
        names.append(("in_g%d" % h, 4096))
    for h in range(2):
        names.append(("out%d" % h, 4096))
    for s in range(12):
        names.append(("ffin%d" % s, 4096))
    for m in range(8):
        names.append(("ffout%d" % m, 3072))
    for h in range(2):
        names.append(("pleg0_%d" % h, 4096))
    names.append(("plep0", 2048))
    names.append(("kv", 3072))
    for s in range(4):
        names.append(("q%d" % s, 4096))
    for h in range(2):
        names.append(("o%d" % h, 4096))
    for e in range(8):
        for s in range(4):
            names.append(("ein%d_%d" % (e, s), 4096))
        for h in range(2):
            names.append(("eout%d_%d" % (e, h), 4096))
    for h in range(2):
        names.append(("pleg1_%d" % h, 4096))
    names.append(("plep1", 2048))
    tab = {}
    off = 0
    for nm, n in names:
        tab[nm] = (off, n)
        off += 128 * n
    return tab, off


VC = {}
_o = 0
for _nm, _n in [("g_mix0", 8), ("g_ffn0", 8), ("g_ple0", 8), ("g_kv", 8), ("g_mix1", 8), ("g_ffn1", 8),
                ("g_ple1", 8), ("g_final", 8), ("conv_w", 32), ("conv_b", 8), ("b_ga", 8), ("b_gx", 8),
                ("lam", 8), ("freq", 1), ("sign", 1), ("carry", 1), ("omc", 1)]:
    VC[_nm] = _o
    _o += _n
NV = _o


def build_vecs(inp, carry):
    v = np.zeros((128, NV), np.float32)

    def col(x):
        return np.asarray(x, np.float32).reshape(8, 128).T

    v[:, VC["g_mix0"]:VC["g_mix0"] + 8] = col(inp["g_mix"][0])
    v[:, VC["g_ffn0"]:VC["g_ffn0"] + 8] = col(inp["g_ffn"][0])
    v[:, VC["g_ple0"]:VC["g_ple0"] + 8] = col(inp["g_ple"][0])
    v[:, VC["g_kv"]:VC["g_kv"] + 8] = col(inp["g_kv"])
    v[:, VC["g_mix1"]:VC["g_mix1"] + 8] = col(inp["g_mix"][1])
    v[:, VC["g_ffn1"]:VC["g_ffn1"] + 8] = col(inp["g_ffn"][1])
    v[:, VC["g_ple1"]:VC["g_ple1"] + 8] = col(inp["g_ple"][1])
    v[:, VC["g_final"]:VC["g_final"] + 8] = col(inp["g_final"])
    for k in range(4):
        v[:, VC["conv_w"] + 8 * k:VC["conv_w"] + 8 * k + 8] = col(inp["lru_conv_w"][0][k])
    v[:, VC["conv_b"]:VC["conv_b"] + 8] = col(inp["lru_conv_b"][0])
    v[:, VC["b_ga"]:VC["b_ga"] + 8] = col(inp["b_lru_ga"][0])
    v[:, VC["b_gx"]:VC["b_gx"] + 8] = col(inp["b_lru_gx"][0])
    v[:, VC["lam"]:VC["lam"] + 8] = col(inp["lru_lambda"][0])
    freqs = (np.float32(500000.0) ** (-np.arange(0, 16, 2, dtype=np.float32) / np.float32(16))).astype(np.float32)
    for p in range(128):
        d = p % 64
        if d < 16:
            v[p, VC["freq"]] = freqs[d % 8]
            v[p, VC["sign"]] = -1.0 if d < 8 else 1.0
    v[:, VC["carry"]] = carry
    v[:, VC["omc"]] = 1.0 - carry
    return v


def build_program(NPRE, NREAL, taps=()):
    nc = bass.Bass("TRN2", target_bir_lowering=False)
    P = Prog(nc)
    NT_ALL = NPRE + NREAL
    TOK = NT_ALL * T
    TOKP = (1 + NREAL) * T
    stab, wtotal = slab_table()
    WROWS = wtotal // 2048

    xT = nc.dram_tensor("xT", [D, TOK], F32, kind="ExternalInput")
    pT = nc.dram_tensor("pT", [2, 256, TOKP], F32, kind="ExternalInput")
    posd = nc.dram_tensor("pos", [1, TOKP], I32, kind="ExternalInput")
    wflat = nc.dram_tensor("wflat", [WROWS, 2048], F32, kind="ExternalInput")
    vecsd = nc.dram_tensor("vecs", [128, NV], F32, kind="ExternalInput")
    identd = nc.dram_tensor("ident", [128, 128], F32, kind="ExternalInput")
    maskd = nc.dram_tensor("maskc", [128, 2, 128], F32, kind="ExternalInput")
    sinkd = nc.dram_tensor("sinks", [1, 16], F32, kind="ExternalInput")
    wrd = nc.dram_tensor("wr", [128, 64], F32, kind="ExternalInput")
    brd = nc.dram_tensor("br", [1, 8], F32, kind="ExternalInput")
    outT = nc.dram_tensor("outT", [D, NREAL * T], F32, kind="ExternalOutput")
    wscr = nc.dram_tensor("wscr", [WROWS, 2048], BF16, kind="Internal")
    tapd = {nm: nc.dram_tensor("tap_" + nm, list(shape), F32, kind="ExternalOutput") for nm, shape in taps}

    st = contextlib.ExitStack()

    def sb(name, shape, dt):
        return st.enter_context(nc.sbuf_tensor(name, shape, dt))

    with st:
        X = [sb("X%d" % i, [128, 8, T], F32) for i in range(2)]
        XB_X = [[Buf("X%d_%d" % (i, c)) for c in range(8)] for i in range(2)]
        XN = sb("XN", [128, 8, T], BF16)
        B_XN = [Buf("XN%d" % c) for c in range(8)]
        ring = sb("ring", [128, NSLOT, SLOT_ELEMS], BF16)
        B_ring = [Buf("ring%d" % s) for s in range(NSLOT)]
        PST = [sb("PST%d" % i, [128, 2, 2, T], F32) for i in range(2)]
        B_PST = [Buf("PST%d" % i) for i in range(2)]
        PT = sb("PT", [128, 2, T], BF16)
        B_PT = Buf("PT")
        POSI = [sb("POSI%d" % i, [128, T], I32) for i in range(2)]
        B_POSI = [Buf("POSI%d" % i) for i in range(2)]
        COS = sb("COS", [128, T], F32)
        SIN = sb("SIN", [128, T], F32)
        B_CS = Buf("cossin")
        RT = [sb("RT%d" % i, [128, T], F32) for i in range(3)]
        B_RT = [Buf("RT%d" % i) for i in range(3)]
        RTI = sb("RTI", [128, T], I32)
        B_RTI = Buf("RTI")
        KT = sb("KT", [128, 5 * 128], BF16)
        B_KT = [Buf("KT%d" % i) for i in range(5)]
        VT = sb("VT", [128, 5, 128], BF16)
        B_VT = [Buf("VT%d" % i) for i in range(5)]
        XBh = sb("XBh", [128, 8, 4 + T], BF16)
        B_XBh = [Buf("XBh%d" % c) for c in range(8)]
        RS = sb("RS", [128, T], F32)
        B_RS = Buf("RS")
        dummy = sb("dmy_sq", [128, 2], F32)
        B_dummy = Buf("dummy")
        HS = sb("HS", [128, 8], F32)
        B_HS = [Buf("HS%d" % c) for c in range(8)]
        vecs = sb("vecs_sb", [128, NV], F32)
        B_vecs = Buf("vecs")
        ident = sb("ident_sb", [128, 128], F32)
        B_ident = Buf("ident")
        ones_b = sb("ones_b", [128, 128], BF16)
        ones_f = sb("ones_f", [128, 128], F32)
        B_ones = Buf("ones")
        DG = sb("DG", [128, 8, 4, 128], BF16)
        B_DG = Buf("DG")
        lruc = sb("lruc", [128, 4, 8], F32)
        B_lruc = Buf("lruc")
        maskf = sb("maskf", [128, 2, 128], F32)
        B_mask = Buf("mask")
        sinkb = sb("sinkb", [128, 16], F32)
        ESINK2 = sb("ESINK2", [128, 4, 256], F32)
        ident_b = sb("ident_b", [128, 128], BF16)
        MASKB = sb("MASKB", [128, 2, T], BF16)
        B_esink = Buf("esink")
        wr_f = sb("wr_f", [128, 64], F32)
        wr_b = sb("wr_b", [128, 64], BF16)
        B_wr = Buf("wr")
        brt = sb("brt", [128, 4, 8], F32)
        B_brt = Buf("brt")
        RL = sb("RL", [128, 4, 8], F32)
        RL2 = sb("RL2", [128, 4, 8], F32)
        REQ1 = sb("REQ1", [128, 4, 8], F32)
        REQ2 = sb("REQ2", [128, 4, 8], F32)
        RGT = sb("RGT", [128, 4, 8], F32)
        RM = sb("RM", [128, 4, 4], F32)
        B_rt_small = Buf("router_small")
        DE = [sb("DE%d" % i, [128, 4, 128], F32) for i in range(2)]
        B_DE = [Buf("DE%d" % i) for i in range(2)]
        NPAGE = 24
        AR = sb("AR", [128, NPAGE * 2048], U8)
        B_pg = [Buf("pg%d" % i) for i in range(NPAGE)]

        def pview(page, dt, nelem, off_bytes=0):
            esz = 4 if dt in (F32, I32) else 2
            b0 = page * 2048 + off_bytes
            return AR[:, b0:b0 + nelem * esz].bitcast(dt)

        def f32pg(page):
            return pview(page, F32, T), [B_pg[page]]

        def bf16half(page, half):
            return pview(page, BF16, T, half * 1024), [B_pg[page]]

        banks = [st.enter_context(nc.psum_tensor("bank%d" % i, [128, T], F32)) for i in range(8)]
        B_bank = [Buf("bank%d" % i) for i in range(8)]
        bank_ctr = [0]

        def nb():
            i = bank_ctr[0] % 8
            bank_ctr[0] += 1
            return banks[i], B_bank[i]

        def vcol(name, c=0):
            j = VC[name] + c
            return vecs[:, j:j + 1]

        def mm(out, lhsT, rhs, start, stop, reads, writes):
            return P.op("pe", lambda e: e.matmul(out, lhsT, rhs, start=start, stop=stop), reads=reads, writes=writes)

        def act(out, in_, func, reads, writes, scale=None, bias=None):
            kw = {}
            if scale is not None:
                kw["scale"] = scale
            if bias is not None:
                kw["bias"] = bias
            return P.op("act", lambda e: e.activation(out=out, in_=in_, func=func, **kw), reads=reads, writes=writes)

        def tt(eng, out, in0, in1, op, reads, writes):
            return P.op(eng, lambda e: e.tensor_tensor(out=out, in0=in0, in1=in1, op=op), reads=reads, writes=writes)

        def ts(eng, out, in0, s1, op0, reads, writes, s2=None, op1=None):
            if op1 is None:
                return P.op(eng, lambda e: e.tensor_scalar(out=out, in0=in0, scalar1=s1, scalar2=None, op0=op0), reads=reads, writes=writes)
            return P.op(eng, lambda e: e.tensor_scalar(out=out, in0=in0, scalar1=s1, scalar2=s2, op0=op0, op1=op1), reads=reads, writes=writes)

        def stt(out, in0, scalar, in1, op0, op1, reads, writes):
            return P.op("dve", lambda e: e.scalar_tensor_tensor(out=out, in0=in0, scalar=scalar, in1=in1, op0=op0, op1=op1), reads=reads, writes=writes)

        def cp(eng, out, in_, reads, writes):
            return P.op(eng, lambda e: e.tensor_copy(out=out, in_=in_), reads=reads, writes=writes)

        def memset(eng, ap, val, writes):
            return P.op(eng, lambda e: e.memset(ap, val), writes=writes)

        def scan(out, d0, d1, init, reads, writes):
            return P.op("dve", lambda e: e.tensor_tensor_scan(out=out, data0=d0, data1=d1, initial=init, op0=ALU.mult, op1=ALU.add),
                        reads=reads, writes=writes)

        def rfast(ap, bufs):
            return P.op("dve", lambda e: e.reciprocal_approx_fast(out=ap, in_=ap), reads=bufs, writes=bufs)

        def recip(ap, bufs):
            return P.op("dve", lambda e: e.reciprocal(out=ap, in_=ap), reads=bufs, writes=bufs)

        tap_ops = []

        def tap(name, ap, reads):
            if name in tapd:
                tap_ops.append(P.op("sp", lambda e: e.dma_start(out=tapd[name].ap(), in_=ap), reads=reads, dma_key="tap_" + name))

        CH = 256
        conv_chunks = []
        r0 = 0
        while r0 < WROWS:
            r1 = min(WROWS, r0 + CH)
            conv_chunks.append((r0, r1))
            r0 = r1
        B_conv = [Buf("conv%d" % i) for i in range(len(conv_chunks))]
        conv_ops = []
        conv_gate = []

        def emit_conversions(lo, hi, gate=()):
          for i in range(lo, min(hi, len(conv_chunks))):
            a, b = conv_chunks[i]
            ed = [conv_ops[i - 2]] if i >= 2 else []
            if i == N_EARLY:
                ed = ed + conv_gate
            if i < lo + 2:
                ed = ed + list(gate)
            o = P.op("pool", (lambda a, b: lambda e: e.dma_start(out=wscr.ap()[a:b, :], in_=wflat.ap()[a:b, :]))(a, b),
                     writes=[B_conv[i]], dma_key="conv%d" % (i % 4), extra_deps=ed)
            conv_ops.append(o)

        N_EARLY = 3
        emit_conversions(0, N_EARLY)

        slot_ctr = [0]

        sticky_cache = {}

        def load_slab(name, sticky=False):
            if sticky and name in sticky_cache:
                return sticky_cache[name]
            r_ = load_slab_(name)
            if sticky:
                sticky_cache[name] = r_
            return r_

        def load_slab_(name):
            off, n = stab[name]
            s = slot_ctr[0] % NSLOT
            slot_ctr[0] += 1
            rlo = off // 2048
            rhi = (off + 128 * n - 1) // 2048
            cbufs = [B_conv[i] for i in range(rlo // CH, rhi // CH + 1)]
            src = bass.AP(wscr, off, [[n, 128], [1, n]])
            o_ = P.op("sp", lambda e: e.dma_start(out=ring[:, s, 0:n], in_=src), reads=cbufs, writes=[B_ring[s]], dma_key="ring%d" % s)
            if len(conv_ops) <= N_EARLY:
                conv_gate.append(o_)
            return s, B_ring[s]

        P.op("sp", lambda e: e.dma_start(out=vecs[:], in_=vecsd.ap()), writes=[B_vecs], dma_key="c_vecs")
        P.op("sp", lambda e: e.dma_start(out=ident[:], in_=identd.ap()), writes=[B_ident], dma_key="c_ident")
        P.op("sp", lambda e: e.dma_start(out=maskf[:], in_=maskd.ap()), writes=[B_mask], dma_key="c_mask")
        P.op("sp", lambda e: e.dma_start(out=sinkb[:], in_=bass.AP(sinkd, 0, [[0, 128], [1, 16]])), writes=[B_esink], dma_key="c_sink")
        P.op("sp", lambda e: e.dma_start(out=wr_f[:], in_=wrd.ap()), writes=[B_wr], dma_key="c_wr")
        P.op("sp", lambda e: e.dma_start(out=brt[:], in_=bass.AP(brd, 0, [[0, 128], [0, 4], [1, 8]])), writes=[B_brt], dma_key="c_br")

        P.op("dve", lambda e: e.memset(dummy[:], 1.0), writes=[B_dummy])
        P.op("dve", lambda e: e.memset(ones_f[:], 1.0), writes=[B_ones])
        P.op("dve", lambda e: e.memset(ones_b[:], 1.0), writes=[B_ones])
        P.op("dve", lambda e: e.memset(HS[:], 0.0), writes=B_HS)
        P.op("dve", lambda e: e.memset(XBh[:, :, 0:4], 0.0), writes=B_XBh)
        cp("dve", wr_b[:], wr_f[:], [B_wr], [B_wr])
        for c in range(8):
            for k in range(4):
                ts("dve", DG[:, c, k, :], ident[:], vcol("conv_w", 8 * k + c), ALU.mult, [B_ident, B_vecs], [B_DG])
        lam = vecs[:, VC["lam"]:VC["lam"] + 8]
        ts("dve", lruc[:, 3, :], lam, -1.0, ALU.mult, [B_vecs], [B_lruc])
        tt("dve", lruc[:, 2, :], lruc[:, 3, :], lam, ALU.max, [B_vecs, B_lruc], [B_lruc])
        act(lruc[:, 3, :], lruc[:, 2, :], AF.Exp, [B_lruc], [B_lruc], scale=-1.0)
        act(lruc[:, 2, :], lruc[:, 3, :], AF.Ln, [B_lruc], [B_lruc], bias=1.0)
        ts("dve", lruc[:, 3, :], lam, -1.0, ALU.mult, [B_vecs, B_lruc], [B_lruc], s2=0.0, op1=ALU.max)
        tt("dve", lruc[:, 3, :], lruc[:, 3, :], lruc[:, 2, :], ALU.add, [B_lruc], [B_lruc])
        ts("dve", lruc[:, 0, :], lruc[:, 3, :], -4.0, ALU.mult, [B_lruc], [B_lruc])
        ts("dve", lruc[:, 1, :], lruc[:, 3, :], -8.0, ALU.mult, [B_lruc], [B_lruc])
        cp("dve", ident_b[:], ident[:], [B_ident], [B_mask])
        for h in range(2):
            ts("dve", MASKB[:, 0, h * 128:(h + 1) * 128], maskf[:, 0, :], -1.0, ALU.add, [B_mask], [B_mask], s2=30000.0, op1=ALU.mult)
            ts("dve", MASKB[:, 0, 256 + h * 128:256 + (h + 1) * 128], maskf[:, 1, :], -1.0, ALU.add, [B_mask], [B_mask], s2=30000.0, op1=ALU.mult)
            ts("dve", MASKB[:, 1, 256 + h * 128:256 + (h + 1) * 128], maskf[:, 1, :], -1.0, ALU.add, [B_mask], [B_mask], s2=30000.0, op1=ALU.mult)
        ts("dve", maskf[:, 0, :], maskf[:, 0, :], vcol("carry"), ALU.mult, [B_mask, B_vecs], [B_mask])
        for h in range(2):
            ts("dve", MASKB[:, 1, h * 128:(h + 1) * 128], maskf[:, 0, :], -1.0, ALU.add, [B_mask], [B_mask], s2=30000.0, op1=ALU.mult)
        act(sinkb[:], sinkb[:], AF.Exp, [B_esink], [B_esink])
        for g in range(4):
            c0 = 2 * g
            for hh in range(2):
                ts("dve", ESINK2[0:64, g, hh * 128:(hh + 1) * 128], ones_f[0:64, :], sinkb[0:64, c0 + hh:c0 + hh + 1], ALU.mult, [B_ones, B_esink], [B_esink])
                ts("dve", ESINK2[64:128, g, hh * 128:(hh + 1) * 128], ones_f[64:128, :], sinkb[64:128, 8 + c0 + hh:9 + c0 + hh], ALU.mult, [B_ones, B_esink], [B_esink])

        last_x_load = [None]

        def issue_loads(ti):
            xb = ti % 2
            src = xT.ap().rearrange("(c p) t -> p c t", p=128)[:, :, ti * T:(ti + 1) * T]
            o_ = P.op("sp", lambda e: e.dma_start(out=X[xb][:], in_=src), writes=XB_X[xb], dma_key="x%d" % xb)
            last_x_load[0] = o_
            if len(conv_ops) <= N_EARLY:
                conv_gate.append(o_)
            if ti >= NPRE - 1:
                pj = ti - (NPRE - 1)
                psrc = pT.ap().rearrange("l (c p) t -> p l c t", p=128)[:, :, :, pj * T:(pj + 1) * T]
                P.op("sp", lambda e: e.dma_start(out=PST[xb][:], in_=psrc), writes=[B_PST[xb]], dma_key="p%d" % xb)
                possrc = bass.AP(posd, pj * T, [[0, 128], [1, T]])
                P.op("sp", lambda e: e.dma_start(out=POSI[xb][:], in_=possrc), writes=[B_POSI[xb]], dma_key="pos%d" % xb)

        def rmsnorm(xb, gname, final=False):
            Xt, BX = X[xb], XB_X[xb]
            bk, Bbk = nb()
            for c in range(8):
                if final:
                    sq, Bsq = bf16half(8 + c // 2, c % 2)
                else:
                    sq, Bsq = XN[:, c, :], [B_XN[c]]
                act(sq, Xt[:, c, :], AF.Square, [BX[c]], Bsq)
                mm(bk[:], ones_b[:], sq, c == 0, c == 7, [B_ones] + Bsq, [Bbk])
            rs, Brs = RS[:], [B_RS]
            act(rs, bk[:], AF.Ln, [Bbk], Brs, scale=1.0 / D, bias=EPS)
            act(rs, rs, AF.Exp, Brs, Brs, scale=-0.5)
            if final:
                for c in range(8):
                    tt("pool", Xt[:, c, :], Xt[:, c, :], rs, ALU.mult, [BX[c]] + Brs, [BX[c]])
                    ts("pool", Xt[:, c, :], Xt[:, c, :], vcol(gname, c), ALU.mult, [BX[c], B_vecs], [BX[c]], s2=1.0, op1=ALU.mult)
                return
            mm(bk[:], ident[:], rs, True, True, [B_ident] + Brs, [Bbk])
            for c in range(8):
                stt(XN[:, c, :], Xt[:, c, :], vcol(gname, c), bk[:], ALU.mult, ALU.mult, [BX[c], B_vecs, Bbk], [B_XN[c]])

        def presqrt():
            act(dummy[:, 0:1], dummy[:, 1:2], AF.Ln, [B_dummy], [B_dummy])

        def proj_add(xb, slab_names, src, Bsrc, kc_n=8):
            Xt, BX = X[xb], XB_X[xb]
            for h, nm in enumerate(slab_names):
                s, Bs = load_slab(nm)
                for mi in range(4):
                    m = 4 * h + mi
                    bk, Bbk = nb()
                    for kc in range(kc_n):
                        mm(W(bk), ring[:, s, kc * 512 + mi * 128: kc * 512 + (mi + 1) * 128], W(src(kc)),
                           kc == 0, kc == kc_n - 1, [Bs] + Bsrc(kc), [Bbk])
                    tt("dve", W(Xt[:, m, :]), W(bk), W(Xt[:, m, :]), ALU.add, [Bbk, BX[m]], [BX[m]])

        CW = [0, T]

        def W(ap):
            return ap[:, CW[0]:CW[1]]

        pre_normed = set()
        ALT_OK = (NPRE - 1 >= 1) and NSLOT >= 5
        B_alt = [Buf("alt%d" % j) for j in range(16)]

        def alt_view(j):
            if j < 8:
                sl, k = 3 + j // 4, j % 4
                return ring[:, sl, k * 1024:(k + 1) * 1024].bitcast(F32), [B_alt[j]]
            j2 = j - 8
            return PST[j2 // 4][:, (j2 % 4) // 2, j2 % 2, :], [B_alt[j]]

        def alt_fence():
            P.op("dve", lambda e: e.memset(dummy[:, 0:1], 1.0), reads=B_alt, writes=[B_ring[3], B_ring[4], B_PST[0], B_PST[1], B_dummy])

        def lru(xb, full, first_mode, normed=False, pool_ok=False):
            Xt, BX = X[xb], XB_X[xb]
            if not normed:
                rmsnorm(xb, "g_mix0")
            for h in range(2):
                s, Bs = load_slab("in_x%d" % h, sticky=not full)
                for mi in range(4):
                    c = 4 * h + mi
                    bk, Bbk = nb()
                    for kc in range(8):
                        mm(bk[:], ring[:, s, kc * 512 + mi * 128: kc * 512 + (mi + 1) * 128], XN[:, kc, :],
                           kc == 0, kc == 7, [Bs, B_XN[kc]], [Bbk])
                    if pool_ok:
                        act(XBh[:, c, 4:4 + T], bk[:], AF.Copy, [Bbk], [B_XBh[c]])
                    else:
                        cp("dve", XBh[:, c, 4:4 + T], bk[:], [Bbk], [B_XBh[c]])
            GATEv = [bf16half(c // 2, c % 2) for c in range(8)]
            def gate_branch():
                for h in range(2):
                    s, Bs = load_slab("in_g%d" % h)
                    for mi in range(4):
                        c = 4 * h + mi
                        bk, Bbk = nb()
                        for kc in range(8):
                            mm(W(bk), ring[:, s, kc * 512 + mi * 128: kc * 512 + (mi + 1) * 128], W(XN[:, kc, :]),
                               kc == 0, kc == 7, [Bs, B_XN[kc]], [Bbk])
                        act(W(GATEv[c][0]), W(bk), AF.Gelu_apprx_tanh, [Bbk], GATEv[c][1])
            sg, Bsg = load_slab("gates", sticky=not full)
            XCbv = [bf16half(4 + c // 2, c % 2) for c in range(8)]
            for half in range(2):
                cs = [4 * half + i for i in range(4)]
                if half == 1 and not full and ALT_OK:
                    XCf = {c: alt_view(0 + i) for i, c in enumerate(cs)}
                    Ip = {c: alt_view(4 + i) for i, c in enumerate(cs)}
                    Av = {c: alt_view(8 + i) for i, c in enumerate(cs)}
                    Mv = {c: alt_view(12 + i) for i, c in enumerate(cs)}
                else:
                    XCf = {c: f32pg(8 + i) for i, c in enumerate(cs)}
                    Ip = {c: f32pg(12 + i) for i, c in enumerate(cs)}
                    Av = {c: f32pg(16 + i) for i, c in enumerate(cs)}
                    Mv = {c: f32pg(20 + i) for i, c in enumerate(cs)}
                for c in cs:
                    bk, Bbk = nb()
                    for k in range(4):
                        mm(bk[:], DG[:, c, k, :], XBh[:, c, 4 - k:4 - k + T], k == 0, k == 3, [B_DG, B_XBh[c]], [Bbk])
                    act(XCf[c][0], bk[:], AF.Identity, [Bbk, B_vecs], XCf[c][1], bias=vcol("conv_b", c))
                    if pool_ok:
                        cp("pool", XCbv[c][0], XCf[c][0], XCf[c][1], XCbv[c][1])
                    else:
                        cp("dve", XCbv[c][0], XCf[c][0], XCf[c][1], XCbv[c][1])
                    cp("dve", XBh[:, c, 1:4], XBh[:, c, T + 1:T + 4], [B_XBh[c]], [B_XBh[c]])
                Rp = {}
                for oc in cs:
                    hb, jh = oc // 2, oc % 2
                    for g in range(2):
                        bk, Bbk = nb()
                        for i in range(2):
                            base = ((g * 4 + hb) * 2 + i) * 256 + jh * 128
                            mm(bk[:], ring[:, sg, base:base + 128], XCbv[2 * hb + i][0], i == 0, i == 1,
                               [Bsg] + XCbv[2 * hb + i][1], [Bbk])
                        if g == 0:
                            act(Mv[oc][0], bk[:], AF.Tanh, [Bbk, B_lruc], Mv[oc][1], scale=0.5, bias=hbias(0, oc))
                        else:
                            act(Ip[oc][0], bk[:], AF.Tanh, [Bbk, B_lruc], Ip[oc][1], scale=0.5, bias=hbias(1, oc))
                for c in cs:
                    act(Av[c][0], Mv[c][0], AF.Exp, Mv[c][1] + [B_lruc], Av[c][1], scale=lruc[:, 0, c:c + 1], bias=lruc[:, 0, c:c + 1])
                for c in cs:
                    if pool_ok:
                        tt("pool", Mv[c][0], Av[c][0], Av[c][0], ALU.mult, Av[c][1], Mv[c][1])
                    else:
                        act(Mv[c][0], Mv[c][0], AF.Exp, Mv[c][1] + [B_lruc], Mv[c][1], scale=lruc[:, 1, c:c + 1], bias=lruc[:, 1, c:c + 1])
                for c in cs:
                    act(Mv[c][0], Mv[c][0], AF.Ln, Mv[c][1], Mv[c][1], scale=-1.0, bias=1.0)
                for c in cs:
                    act(Mv[c][0], Mv[c][0], AF.Exp, Mv[c][1], Mv[c][1], scale=0.5)
                if half == 0 and full:
                    gate_branch()
                for c in cs:
                    if first_mode == "one":
                        memset("dve", Mv[c][0][:, 0:1], 1.0, Mv[c][1])
                    elif first_mode == "blend":
                        ts("dve", Mv[c][0][:, 0:1], Mv[c][0][:, 0:1], vcol("carry"), ALU.mult, Mv[c][1] + [B_vecs], Mv[c][1],
                           s2=vcol("omc"), op1=ALU.add)
                        ts("dve", HS[:, c:c + 1], HS[:, c:c + 1], vcol("carry"), ALU.mult, [B_HS[c], B_vecs], [B_HS[c]])
                    stt(Ip[c][0], Ip[c][0], 1.0, XCf[c][0], ALU.add, ALU.mult, Ip[c][1] + XCf[c][1], Ip[c][1])
                    stt(Ip[c][0], Ip[c][0], 0.5, Mv[c][0], ALU.mult, ALU.mult, Ip[c][1] + Mv[c][1], Ip[c][1])
                    scan(XCf[c][0], Av[c][0], Ip[c][0], HS[:, c:c + 1], Av[c][1] + Ip[c][1] + [B_HS[c]], XCf[c][1])
                    cp("dve", HS[:, c:c + 1], XCf[c][0][:, T - 1:T], XCf[c][1], [B_HS[c]])
                    if full:
                        tt("pool" if pool_ok else "dve", W(XBh[:, c, 4:4 + T]), W(XCf[c][0]), W(GATEv[c][0]), ALU.mult, XCf[c][1] + GATEv[c][1], [B_XBh[c]])
            if full:
                proj_add(xb, ["out0", "out1"], lambda kc: XBh[:, kc, 4:4 + T], lambda kc: [B_XBh[kc]])

        def hbias(g, oc):
            return hb_tab[:, g, oc:oc + 1]

        hb_tab = sb("hb_tab", [128, 2, 8], F32)
        ts("dve", hb_tab[:, 0, :], vecs[:, VC["b_ga"]:VC["b_ga"] + 8], 0.5, ALU.mult, [B_vecs], [B_lruc])
        ts("dve", hb_tab[:, 1, :], vecs[:, VC["b_gx"]:VC["b_gx"] + 8], 0.5, ALU.mult, [B_vecs], [B_lruc])

        def ffn(xb):
            rmsnorm(xb, "g_ffn0")
            HID = [bf16half(j // 2, j % 2) for j in range(24)]
            for s12 in range(12):
                s, Bs = load_slab("ffin%d" % s12)
                for jj in range(2):
                    j = 2 * s12 + jj
                    bg, Bbg = nb()
                    bu, Bbu = nb()
                    for kc in range(8):
                        mm(W(bg), ring[:, s, kc * 512 + jj * 128: kc * 512 + (jj + 1) * 128], W(XN[:, kc, :]), kc == 0, kc == 7, [Bs, B_XN[kc]], [Bbg])
                    for kc in range(8):
                        mm(W(bu), ring[:, s, kc * 512 + (2 + jj) * 128: kc * 512 + (3 + jj) * 128], W(XN[:, kc, :]), kc == 0, kc == 7, [Bs, B_XN[kc]], [Bbu])
                    sgv, Bsgv = f32pg(12 + (j % 4))
                    act(W(sgv), W(bg), AF.Silu, [Bbg], Bsgv)
                    tt("dve", W(HID[j][0]), W(bu), W(sgv), ALU.mult, [Bbu] + Bsgv, HID[j][1])
                if s12 == 5:
                    rope_sin()
            presqrt()
            Xt, BX = X[xb], XB_X[xb]
            for m in range(8):
                s, Bs = load_slab("ffout%d" % m)
                bk, Bbk = nb()
                for j in range(24):
                    mm(W(bk), ring[:, s, j * 128:(j + 1) * 128], W(HID[j][0]), j == 0, j == 23, [Bs] + HID[j][1], [Bbk])
                tt("dve", W(Xt[:, m, :]), W(bk), W(Xt[:, m, :]), ALU.add, [Bbk, BX[m]], [BX[m]])

        def ple(xb, layer):
            Xt, BX = X[xb], XB_X[xb]
            rmsnorm(xb, "g_ple%d" % layer)
            cp("dve", PT[:], PST[xb][:, layer, :, :], [B_PST[xb]], [B_PT])
            GP = [f32pg(m) for m in range(8)]
            for h in range(2):
                s, Bs = load_slab("pleg%d_%d" % (layer, h))
                for mi in range(4):
                    m = 4 * h + mi
                    bk, Bbk = nb()
                    for kc in range(8):
                        mm(W(bk), ring[:, s, kc * 512 + mi * 128: kc * 512 + (mi + 1) * 128], W(XN[:, kc, :]), kc == 0, kc == 7, [Bs, B_XN[kc]], [Bbk])
                    act(W(GP[m][0]), W(bk), AF.Sigmoid, [Bbk], GP[m][1])
            presqrt()
            s, Bs = load_slab("plep%d" % layer)
            for m in range(8):
                bk, Bbk = nb()
                for kc in range(2):
                    mm(W(bk), ring[:, s, kc * 1024 + m * 128: kc * 1024 + (m + 1) * 128], W(PT[:, kc, :]), kc == 0, kc == 1, [Bs, B_PT], [Bbk])
                tt("dve", W(GP[m][0]), W(bk), W(GP[m][0]), ALU.mult, [Bbk] + GP[m][1], GP[m][1])
                tt("dve", W(Xt[:, m, :]), W(GP[m][0]), W(Xt[:, m, :]), ALU.add, GP[m][1] + [BX[m]], [BX[m]])

        def rope_tables(xb):
            a0, a1, a2 = RT[0], RT[1], RT[2]
            Ba = B_RT
            cp("dve", a0[:], POSI[xb][:], [B_POSI[xb]], [Ba[0]])
            ts("dve", a0[:], a0[:], vcol("freq"), ALU.mult, [Ba[0], B_vecs], [Ba[0]])
            ts("dve", RTI[:], a0[:], 1.0 / (2 * PI), ALU.mult, [Ba[0]], [B_RTI])
            cp("dve", a1[:], RTI[:], [B_RTI], [Ba[1]])
            C1 = 6.28125
            C2 = float(2 * np.pi - 6.28125)
            stt(a0[:], a1[:], -C1, a0[:], ALU.mult, ALU.add, [Ba[1], Ba[0]], [Ba[0]])
            stt(a0[:], a1[:], -C2, a0[:], ALU.mult, ALU.add, [Ba[1], Ba[0]], [Ba[0]])
            ts("dve", a0[:], a0[:], PI, ALU.min, [Ba[0]], [Ba[0]], s2=-PI, op1=ALU.max)
            ts("dve", a1[:], a0[:], PI / 2, ALU.is_gt, [Ba[0]], [Ba[1]])
            stt(a1[:], a1[:], -2 * PI, a0[:], ALU.mult, ALU.add, [Ba[1], Ba[0]], [Ba[1]])
            ts("dve", a1[:], a1[:], PI / 2, ALU.add, [Ba[1]], [Ba[1]], s2=PI, op1=ALU.min)

        def rope_sin():
            act(SIN[:], RT[0][:], AF.Sin, [B_RT[0], B_vecs], [B_CS], scale=vcol("sign"))
            act(COS[:], RT[1][:], AF.Sin, [B_RT[1]], [B_CS])

        def kv(xb):
            rmsnorm(xb, "g_kv")
            s, Bs = load_slab("kv")
            bk, Bbk = nb()
            bk2, Bbk2 = nb()
            for kc in range(8):
                mm(W(bk), ring[:, s, kc * 384: kc * 384 + 128], W(XN[:, kc, :]), kc == 0, kc == 7, [Bs, B_XN[kc]], [Bbk])
            for kc in range(8):
                mm(W(bk2), ring[:, s, kc * 384 + 128: kc * 384 + 256], W(XN[:, kc, :]), kc == 0, kc == 7, [Bs, B_XN[kc]], [Bbk2])
            t1, Bt1 = f32pg(0)
            t2, Bt2 = f32pg(1)
            tt("dve", W(t1), W(bk), W(COS), ALU.mult, [Bbk, B_CS], Bt1)
            tt("dve", W(t2), W(bk2), W(SIN), ALU.mult, [Bbk2, B_CS], Bt2)
            tt("dve", KT[:, 128 + CW[0]:128 + CW[1]], W(t1), W(t2), ALU.add, Bt1 + Bt2, B_KT[1:5])
            bv, Bbv = nb()
            b0 = CW[0] // 128
            for blk in range(b0, 4):
                for kc in range(8):
                    mm(bv[:, blk * 128:(blk + 1) * 128], XN[:, kc, blk * 128:(blk + 1) * 128], ring[:, s, kc * 384 + 256: kc * 384 + 384],
                       kc == 0, kc == 7, [Bs, B_XN[kc]], [Bbv])
            act(VT[:, 1 + b0:5, :], bv[:, b0 * 128:512].rearrange("p (b f) -> p b f", b=4 - b0), AF.Copy, [Bbv], B_VT[1:5])

        def kv_shift():
            cp("dve", KT[:, 0:128], KT[:, 512:640], [B_KT[4]], [B_KT[0]])
            cp("dve", VT[:, 0, :], VT[:, 4, :], [B_VT[4]], [B_VT[0]])

        def attention(xb, first_real):
            Xt, BX = X[xb], XB_X[xb]
            rmsnorm(xb, "g_mix1")
            QR = [bf16half(4 + c // 2, c % 2) for c in range(8)]
            for s4 in range(4):
                s, Bs = load_slab("q%d" % s4)
                for ci in range(2):
                    c = 2 * s4 + ci
                    bq, Bbq = nb()
                    bq2, Bbq2 = nb()
                    for kc in range(8):
                        mm(bq[:], ring[:, s, kc * 512 + (2 * ci) * 128: kc * 512 + (2 * ci + 1) * 128], XN[:, kc, :], kc == 0, kc == 7, [Bs, B_XN[kc]], [Bbq])
                    for kc in range(8):
                        mm(bq2[:], ring[:, s, kc * 512 + (2 * ci + 1) * 128: kc * 512 + (2 * ci + 2) * 128], XN[:, kc, :], kc == 0, kc == 7, [Bs, B_XN[kc]], [Bbq2])
                    t1, Bt1 = f32pg(c % 2)
                    t2, Bt2 = f32pg(2 + c % 2)
                    tt("dve", t1, bq[:], COS[:], ALU.mult, [Bbq, B_CS], Bt1)
                    tt("dve", t2, bq2[:], SIN[:], ALU.mult, [Bbq2, B_CS], Bt2)
                    tt("pool", QR[c][0], t1, t2, ALU.add, Bt1 + Bt2, QR[c][1])
            ATT = [bf16half(8 + c // 2, c % 2) for c in range(8)]
            ATTall = pview(8, BF16, 8 * T).rearrange("p (c t) -> p c t", c=8)
            gi = 0
            for qb in range(4):
                for g in range(4):
                    c0 = 2 * g
                    par = gi % 2
                    gi += 1
                    EA, BEA = bf16half(12 + par, 0)
                    EB, BEB = bf16half(12 + par, 1)
                    sa, Bsa = nb()
                    sbk, Bsb = nb()
                    zu, Bzu = nb()
                    kprev = KT[:, qb * 128:(qb + 1) * 128]
                    kcur = KT[:, (qb + 1) * 128:(qb + 2) * 128]
                    Bk = [B_KT[qb], B_KT[qb + 1]]
                    mkb = MASKB[:, 1 if (first_real and qb == 0) else 0, :]
                    for base, sbank, Bsbank in ((0, sa, Bsa), (64, sbk, Bsb)):
                        mm(sbank[:], ident_b[:], mkb, True, False, [B_mask], [Bsbank])
                        for jc, ksl in enumerate((kprev, kcur)):
                            for hh in range(2):
                                c = c0 + hh
                                col = (2 * jc + hh) * 128
                                mm(sbank[:, col:col + 128], ksl[base:base + 64, :], QR[c][0][base:base + 64, qb * 128:(qb + 1) * 128],
                                   False, (jc == 1 and hh == 1), Bk + QR[c][1], [Bsbank])
                    act(EA, sa[:], AF.Exp, [Bsa], BEA, scale=0.125)
                    act(EB, sbk[:], AF.Exp, [Bsb], BEB, scale=0.125)
                    Bv = [B_VT[qb], B_VT[qb + 1]]
                    for bi, (Ev, BEv) in enumerate(((EA, BEA), (EB, BEB))):
                        r0 = 64 * bi
                        mm(zu[r0:r0 + 64, 256:512], ones_b[:, 0:64], Ev[:, 0:256], True, False, [B_ones] + BEv, [Bzu])
                        mm(zu[r0:r0 + 64, 256:512], ones_b[:, 0:64], Ev[:, 256:512], False, True, [B_ones] + BEv, [Bzu])
                        for hh in range(2):
                            mm(zu[r0:r0 + 64, hh * 128:(hh + 1) * 128], VT[:, qb, r0:r0 + 64], Ev[:, hh * 128:(hh + 1) * 128], True, False, Bv + BEv, [Bzu])
                            mm(zu[r0:r0 + 64, hh * 128:(hh + 1) * 128], VT[:, qb + 1, r0:r0 + 64], Ev[:, 256 + hh * 128:256 + (hh + 1) * 128], False, True, Bv + BEv, [Bzu])
                    rz = pview(16 + par, F32, 256)
                    Brz = [B_pg[16 + par]]
                    tt("dve", rz, zu[:, 256:512], ESINK2[:, g, :], ALU.add, [Bzu, B_esink], Brz)
                    act(rz, rz, AF.Ln, Brz, Brz)
                    act(rz, rz, AF.Exp, Brz, Brz, scale=-1.0)
                    oall = ATTall[:, c0:c0 + 2, qb * 128:(qb + 1) * 128]
                    Batt = ATT[c0][1]
                    tt("dve", oall, zu[:, 0:256].rearrange("p (h q) -> p h q", h=2), rz.rearrange("p (h q) -> p h q", h=2),
                       ALU.mult, [Bzu] + Brz, Batt)
            presqrt()
            proj_add(xb, ["o0", "o1"], lambda kc: ATT[kc][0], lambda kc: ATT[kc][1])

        def moe(xb):
            Xt, BX = X[xb], XB_X[xb]
            rmsnorm(xb, "g_ffn1")
            bl, Bbl = nb()
            for blk in range(4):
                for kc in range(8):
                    mm(bl[:, blk * 8:(blk + 1) * 8], XN[:, kc, blk * 128:(blk + 1) * 128], wr_b[:, kc * 8:(kc + 1) * 8], kc == 0, kc == 7,
                       [B_XN[kc], B_wr], [Bbl])
            Bs_ = [B_rt_small]
            tt("dve", RL[:], bl[:, 0:32].rearrange("p (b e) -> p b e", b=4), brt[:], ALU.add, [Bbl, B_brt], Bs_)
            P.op("dve", lambda e: e.tensor_reduce(out=RM[:, :, 0], in_=RL[:], axis=AX.X, op=ALU.max), reads=Bs_, writes=Bs_)
            m1b = bass.AP(RM, 0, [[16, 128], [4, 4], [0, 8]])
            m2b = bass.AP(RM, 1, [[16, 128], [4, 4], [0, 8]])
            w1b = bass.AP(RM, 2, [[16, 128], [4, 4], [0, 8]])
            w2b = bass.AP(RM, 3, [[16, 128], [4, 4], [0, 8]])
            tt("dve", REQ1[:], RL[:], m1b, ALU.is_equal, Bs_, Bs_)
            stt(RL2[:], REQ1[:], -1e30, RL[:], ALU.mult, ALU.add, Bs_, Bs_)
            P.op("dve", lambda e: e.tensor_reduce(out=RM[:, :, 1], in_=RL2[:], axis=AX.X, op=ALU.max), reads=Bs_, writes=Bs_)
            tt("dve", REQ2[:], RL2[:], m2b, ALU.is_equal, Bs_, Bs_)
            tt("dve", RM[:, :, 2], RM[:, :, 0], RM[:, :, 1], ALU.subtract, Bs_, Bs_)
            act(RM[:, :, 3], RM[:, :, 2], AF.Tanh, Bs_, Bs_, scale=0.5)
            ts("dve", RM[:, :, 2], RM[:, :, 3], 0.5, ALU.mult, Bs_, Bs_, s2=0.5, op1=ALU.add)
            ts("dve", RM[:, :, 3], RM[:, :, 3], -0.5, ALU.mult, Bs_, Bs_, s2=0.5, op1=ALU.add)
            tt("dve", REQ1[:], REQ1[:], w1b, ALU.mult, Bs_, Bs_)
            tt("dve", REQ2[:], REQ2[:], w2b, ALU.mult, Bs_, Bs_)
            tt("dve", RGT[:], REQ1[:], REQ2[:], ALU.add, Bs_, Bs_)
            GE = [f32pg(e) for e in range(8)]
            identb = bass.AP(ident, 0, [[128, 128], [0, 4], [1, 128]])
            for e in range(8):
                de, Bde = DE[e % 2], B_DE[e % 2]
                gsrc = bass.AP(RGT, e, [[32, 128], [8, 4], [0, 128]])
                tt("dve", de[:], identb, gsrc, ALU.mult, [B_ident] + Bs_, [Bde])
                bk, Bbk = nb()
                mm(bk[:], ones_f[:], de[:].rearrange("p b t -> p (b t)"), True, True, [B_ones, Bde], [Bbk])
                act(GE[e][0], bk[:], AF.Copy, [Bbk], GE[e][1])
            for e in range(8):
                HE = [bf16half(8 + 4 * (e % 2) + j // 2, j % 2) for j in range(8)]
                for s4 in range(4):
                    s, Bs = load_slab("ein%d_%d" % (e, s4))
                    for jj in range(2):
                        j = 2 * s4 + jj
                        bg, Bbg = nb()
                        bu, Bbu = nb()
                        for kc in range(8):
                            mm(bg[:], ring[:, s, kc * 512 + jj * 128: kc * 512 + (jj + 1) * 128], XN[:, kc, :], kc == 0, kc == 7, [Bs, B_XN[kc]], [Bbg])
                        for kc in range(8):
                            mm(bu[:], ring[:, s, kc * 512 + (2 + jj) * 128: kc * 512 + (3 + jj) * 128], XN[:, kc, :], kc == 0, kc == 7, [Bs, B_XN[kc]], [Bbu])
                        sgv, Bsgv = f32pg(16 + (j % 4))
                        sgg, Bsgg = f32pg(20 + (j % 4))
                        act(sgv, bg[:], AF.Silu, [Bbg], Bsgv)
                        tt("pool", sgg, sgv, GE[e][0], ALU.mult, Bsgv + GE[e][1], Bsgg)
                        tt("dve", HE[j][0], bu[:], sgg, ALU.mult, [Bbu] + Bsgg, HE[j][1])
                if e == 7:
                    presqrt()
                proj_add(xb, ["eout%d_0" % e, "eout%d_1" % e], lambda kc: HE[kc][0], lambda kc: HE[kc][1])

        CONV_PER_PRE = 9
        issue_loads(0)
        out_ops = []
        for ti in range(NT_ALL):
            xb = ti % 2
            is_pre = ti < NPRE - 1
            is_halo = ti == NPRE - 1
            ri = ti - NPRE
            first_mode = "one" if ti == 0 else ("blend" if ri == 0 else None)
            if is_pre:
                lru(xb, False, first_mode)
                if ti == NPRE - 2 and ALT_OK:
                    alt_fence()
                if ti + 1 < NT_ALL:
                    issue_loads(ti + 1)
                lo = N_EARLY + ti * CONV_PER_PRE
                hi = len(conv_chunks) if ti == NPRE - 2 else lo + CONV_PER_PRE
                emit_conversions(lo, hi, gate=[last_x_load[0]])
                continue
            if ri >= 0:
                kv_shift()
            CW[0] = (T - 128) if is_halo else 0
            rope_tables(xb)
            lru(xb, True, first_mode, normed=(ti in pre_normed), pool_ok=(ri >= 1))
            if ri == 0:
                tap("x_lru", X[xb][:], XB_X[xb])
            if ti + 1 < NT_ALL:
                issue_loads(ti + 1)
            ffn(xb)
            if ri == 0:
                tap("x_ffn", X[xb][:], XB_X[xb])
            ple(xb, 0)
            if ri == 0:
                tap("x_ple0", X[xb][:], XB_X[xb])
            kv(xb)
            if is_halo:
                continue
            attention(xb, ri == 0)
            if ri == 0:
                tap("x_att", X[xb][:], XB_X[xb])
            moe(xb)
            if ri == 0:
                tap("x_moe", X[xb][:], XB_X[xb])
            ple(xb, 1)
            if ti + 1 < NT_ALL:
                rmsnorm((ti + 1) % 2, "g_mix0")
                pre_normed.add(ti + 1)
            rmsnorm(xb, "g_final", final=True)
            dst = outT.ap().rearrange("(c p) t -> p c t", p=128)[:, :, ri * T:(ri + 1) * T]
            out_ops.append(P.op("pool", (lambda dst, xb: lambda e: e.dma_start(out=dst, in_=X[xb][:]))(dst, xb),
                                reads=XB_X[xb], dma_key="out%d" % xb))
        P.emit(final_waits=out_ops + tap_ops)
    return nc


_CACHE = {}


def _consts():
    ident = np.eye(128, dtype=np.float32)
    j = np.arange(128)[:, None]
    q = np.arange(128)[None, :]
    mask = np.zeros((128, 2, 128), np.float32)
    mask[:, 0, :] = (j > q)
    mask[:, 1, :] = (j <= q)
    return ident, mask


def run(inputs, n_cores, taps=(), trace=False):
    x = np.asarray(inputs["x"], np.float32)
    B, S, _ = x.shape
    HALF = S // 2
    assert B * 2 == n_cores and HALF % T == 0
    NPRE = NREAL = HALF // T
    p = np.asarray(inputs["p"], np.float32)
    pos = np.asarray(inputs["positions"], np.int32)
    slabs = build_slabs({k: np.asarray(v) for k, v in inputs.items()})
    wflat = np.concatenate([a.reshape(-1) for _, a in slabs]).astype(np.float32).reshape(-1, 2048)
    stab, wtotal = slab_table()
    assert wflat.size == wtotal
    ident, mask = _consts()
    wr = np.ascontiguousarray(np.asarray(inputs["w_router"][0], np.float32).reshape(8, 128, 8).transpose(1, 0, 2).reshape(128, 64))
    br = np.asarray(inputs["b_router"][0], np.float32).reshape(1, 8)
    sinks = np.asarray(inputs["attn_sinks"][0], np.float32).reshape(1, 16)
    key = (NPRE, NREAL, tuple(taps))
    if key not in _CACHE:
        _CACHE[key] = build_program(NPRE, NREAL, taps)
    nc = _CACHE[key]
    in_maps = []
    for cid in range(n_cores):
        b, half = cid // 2, cid % 2
        xT = np.zeros((D, 2 * HALF), np.float32)
        pTc = np.zeros((2, 256, T + HALF), np.float32)
        posc = np.zeros((1, T + HALF), np.int32)
        if half == 0:
            xT[:, HALF:] = x[b, 0:HALF].T
            pTc[:, :, T:] = p[:, b, 0:HALF].transpose(0, 2, 1)
            posc[0, T:] = pos[b, 0:HALF]
        else:
            xT[:] = x[b].T
            pTc[:] = p[:, b, HALF - T:S].transpose(0, 2, 1)
            posc[0] = pos[b, HALF - T:S]
        in_maps.append({
            "xT": xT, "pT": pTc, "pos": posc, "wflat": wflat, "vecs": build_vecs(inputs, float(half)),
            "ident": ident, "maskc": mask, "sinks": sinks, "wr": wr, "br": br,
        })
    res = run_bass_kernel_spmd(nc, in_maps, core_ids=list(range(n_cores)), trace=trace)
    out = np.zeros((B, S, D), np.float32)
    for cid in range(n_cores):
        b, half = cid // 2, cid % 2
        out[b, half * HALF:(half + 1) * HALF] = res.results[cid]["outT"].T
    return out, res


def kernel(**inputs):
    out, _ = run(inputs, 8)
    return out
```
